# Optimizing a Trainium2 kernel written in Bass

```python
import math
import jax
import jax.numpy as jnp
from jax import lax
import numpy as np

D_MODEL = 1024
BATCH = 8
SEQ = 4096
DEPTH = 4

CHUNK = 64
N_A_LAYERS = DEPTH // 2
N_B_LAYERS = DEPTH - N_A_LAYERS
EPS = 1e-6
GDN_HEAD_DIM = 128
GDN_HEADS = D_MODEL // GDN_HEAD_DIM
GDN_INNER = GDN_HEADS * GDN_HEAD_DIM
GDN_PROJ = 4 * GDN_INNER + 2 * GDN_HEADS
CONV_WIDTH = 4
FOX_HEAD_DIM = 64
FOX_HEADS = D_MODEL // FOX_HEAD_DIM
FOX_INNER = FOX_HEADS * FOX_HEAD_DIM
Q_BLOCK = 128
N_GROUPS = 4
EXPERTS_PER_GROUP = 8
N_EXPERTS = N_GROUPS * EXPERTS_PER_GROUP
TOP_K = 2
D_EXPERT = D_MODEL // 2
EXPERT_BLOCK = 256

kernel_name = 'yoco_gdn_fox_hier_moe_adaln'


def rms_norm(x, gain):
    xf = x.astype(jnp.float32)
    y = xf * lax.rsqrt(jnp.mean(xf * xf, axis=-1, keepdims=True) + EPS)
    return (y * gain.astype(jnp.float32)).astype(x.dtype)


def l2_norm(x):
    xf = x.astype(jnp.float32)
    return xf * lax.rsqrt(jnp.sum(xf * xf, axis=-1, keepdims=True) + EPS)


def modulate(x, gain, shift, scale):
    return rms_norm(x, gain) * (1 + scale) + shift


def causal_depthwise_conv(x, w):
    width, ch = w.shape
    return lax.conv_general_dilated(
        x, w[:, None, :].astype(x.dtype), window_strides=(1,), padding=[(width - 1, 0)],
        dimension_numbers=('NWC', 'WIO', 'NWC'), feature_group_count=ch)


def chunked_gated_delta_rule(q, k, v, g, beta):
    bsz, seqlen, nh, dk = q.shape
    dv = v.shape[-1]
    n = seqlen // CHUNK

    def blocks(t):
        return t.reshape(bsz, n, CHUNK, nh, -1).transpose(1, 0, 3, 2, 4)

    qc, kc, vc = blocks(q), blocks(k), blocks(v)
    gc = g.reshape(bsz, n, CHUNK, nh).transpose(1, 0, 3, 2)
    bc = beta.reshape(bsz, n, CHUNK, nh).transpose(1, 0, 3, 2)
    gcum = jnp.cumsum(gc, axis=-1)
    incl = jnp.tril(jnp.ones((CHUNK, CHUNK), dtype=bool))
    strict = jnp.tril(jnp.ones((CHUNK, CHUNK), dtype=bool), k=-1)
    decay = jnp.exp(jnp.where(incl, gcum[..., :, None] - gcum[..., None, :], -jnp.inf))
    kbeta = kc * bc[..., None]
    a_mat = jnp.where(strict, jnp.einsum('nbhid,nbhjd->nbhij', kbeta, kc) * decay, 0.0)
    lower = a_mat + jnp.eye(CHUNK, dtype=a_mat.dtype)
    w = lax.linalg.triangular_solve(lower, kbeta * jnp.exp(gcum)[..., None],
                                    left_side=True, lower=True, unit_diagonal=True)
    u = lax.linalg.triangular_solve(lower, vc * bc[..., None],
                                    left_side=True, lower=True, unit_diagonal=True)

    def step(state, inp):
        q_i, k_i, u_i, w_i, g_i, decay_i = inp
        v_new = u_i - jnp.einsum('bhid,bhdv->bhiv', w_i, state)
        o_inter = jnp.einsum('bhid,bhdv->bhiv', q_i * jnp.exp(g_i)[..., None], state)
        attn = jnp.einsum('bhid,bhjd->bhij', q_i, k_i) * decay_i
        o_i = o_inter + jnp.einsum('bhij,bhjv->bhiv', attn, v_new)
        g_last = g_i[..., -1]
        k_dec = k_i * jnp.exp(g_last[..., None] - g_i)[..., None]
        state = state * jnp.exp(g_last)[..., None, None] + jnp.einsum('bhid,bhiv->bhdv', k_dec, v_new)
        return state, o_i

    state0 = jnp.zeros((bsz, nh, dk, dv), jnp.float32)
    _, o = lax.scan(step, state0, (qc, kc, u, w, gcum, decay))
    return o.transpose(1, 0, 3, 2, 4).reshape(bsz, seqlen, nh, dv)


def gdn_mixer(h, w_in, conv_w, a_log, dt_bias, out_norm_g, w_out):
    bsz, seqlen, _ = h.shape
    proj = h @ w_in
    qkv = jax.nn.silu(causal_depthwise_conv(proj[..., :3 * GDN_INNER], conv_w))
    z = proj[..., 3 * GDN_INNER:4 * GDN_INNER]
    a_in = proj[..., 4 * GDN_INNER:4 * GDN_INNER + GDN_HEADS].astype(jnp.float32)
    b_in = proj[..., 4 * GDN_INNER + GDN_HEADS:].astype(jnp.float32)
    q, k, v = jnp.split(qkv, 3, axis=-1)
    q = l2_norm(q.reshape(bsz, seqlen, GDN_HEADS, GDN_HEAD_DIM)) * (GDN_HEAD_DIM ** -0.5)
    k = l2_norm(k.reshape(bsz, seqlen, GDN_HEADS, GDN_HEAD_DIM))
    v = v.reshape(bsz, seqlen, GDN_HEADS, GDN_HEAD_DIM).astype(jnp.float32)
    beta = jax.nn.sigmoid(b_in)
    g = -jnp.exp(a_log.astype(jnp.float32)) * jax.nn.softplus(a_in + dt_bias.astype(jnp.float32))
    o = chunked_gated_delta_rule(q, k, v, g, beta)
    zf = z.reshape(bsz, seqlen, GDN_HEADS, GDN_HEAD_DIM).astype(jnp.float32)
    o = rms_norm(o, out_norm_g) * jax.nn.silu(zf)
    return o.reshape(bsz, seqlen, GDN_INNER).astype(h.dtype) @ w_out


def shared_kv(x_mid, c_act, kv_mod_w, kv_mod_b, kv_norm_g, kv_w, kv_forget_b, k_norm_g):
    bsz, seqlen, _ = x_mid.shape
    shift, scale = jnp.split(c_act @ kv_mod_w + kv_mod_b, 2, axis=-1)
    h = modulate(x_mid, kv_norm_g, shift[:, None, :], scale[:, None, :])
    kvf = h @ kv_w
    k = rms_norm(kvf[..., :FOX_INNER].reshape(bsz, seqlen, FOX_HEADS, FOX_HEAD_DIM), k_norm_g)
    v = kvf[..., FOX_INNER:2 * FOX_INNER].reshape(bsz, seqlen, FOX_HEADS, FOX_HEAD_DIM)
    log_f = jax.nn.log_sigmoid(kvf[..., 2 * FOX_INNER:].astype(jnp.float32) + kv_forget_b.astype(jnp.float32))
    fcum = jnp.cumsum(log_f, axis=1).transpose(0, 2, 1)
    return k.transpose(0, 2, 1, 3), v.transpose(0, 2, 1, 3), fcum


def fox_mixer(h, w_qz, q_norm_g, w_out, k, v, fcum):
    bsz, seqlen, _ = h.shape
    qz = h @ w_qz
    q = rms_norm(qz[..., :FOX_INNER].reshape(bsz, seqlen, FOX_HEADS, FOX_HEAD_DIM), q_norm_g)
    q = q.transpose(0, 2, 1, 3)
    z = qz[..., FOX_INNER:].reshape(bsz, seqlen, FOX_HEADS, FOX_HEAD_DIM)
    scale = FOX_HEAD_DIM ** -0.5
    outs = []
    for blk in range(seqlen // Q_BLOCK):
        lo, hi = blk * Q_BLOCK, (blk + 1) * Q_BLOCK
        s = jnp.einsum('bhqd,bhkd->bhqk', q[:, :, lo:hi], k[:, :, :hi]).astype(jnp.float32) * scale
        s = s + fcum[:, :, lo:hi, None] - fcum[:, :, None, :hi]
        causal = (lo + jnp.arange(Q_BLOCK))[:, None] >= jnp.arange(hi)[None, :]
        p = jax.nn.softmax(jnp.where(causal, s, -jnp.inf), axis=-1)
        outs.append(jnp.einsum('bhqk,bhkd->bhqd', p.astype(v.dtype), v[:, :, :hi]))
    o = jnp.concatenate(outs, axis=2).transpose(0, 2, 1, 3) * jax.nn.sigmoid(z)
    return o.reshape(bsz, seqlen, FOX_INNER) @ w_out


def grouped_expert_mlp(ht, expert_idx, weights, w_gate, w_up, w_down):
    n_tok, d = ht.shape
    m = n_tok * TOP_K
    flat_e = expert_idx.reshape(m)
    order = jnp.argsort(flat_e)
    sorted_e = flat_e[order]
    counts = jnp.bincount(flat_e, length=N_EXPERTS)
    padded = (counts + EXPERT_BLOCK - 1) // EXPERT_BLOCK * EXPERT_BLOCK
    pad_end = jnp.cumsum(padded)
    pad_start = pad_end - padded
    start = jnp.cumsum(counts) - counts
    dest = pad_start[sorted_e] + jnp.arange(m) - start[sorted_e]
    n_blocks = -(-m // EXPERT_BLOCK) + N_EXPERTS
    rows = n_blocks * EXPERT_BLOCK
    row_token = jnp.zeros((rows,), jnp.int32).at[dest].set((order // TOP_K).astype(jnp.int32))
    block_expert = jnp.minimum(
        jnp.searchsorted(pad_end, jnp.arange(n_blocks) * EXPERT_BLOCK, side='right'), N_EXPERTS - 1)
    xb = ht[row_token].reshape(n_blocks, EXPERT_BLOCK, d)

    def expert_block(args):
        xblk, e = args
        return (jax.nn.silu(xblk @ w_gate[e]) * (xblk @ w_up[e])) @ w_down[e]

    yb = lax.map(expert_block, (xb, block_expert)).reshape(rows, d)
    y_assign = jnp.zeros((m, d), yb.dtype).at[order].set(yb[dest])
    return jnp.einsum('tkd,tk->td', y_assign.reshape(n_tok, TOP_K, d), weights.astype(yb.dtype))


def hier_moe(h, w_group, b_group, w_expert, b_expert, w_gate, w_up, w_down):
    bsz, seqlen, d = h.shape
    n_tok = bsz * seqlen
    ht = h.reshape(n_tok, d)
    group_logits = (ht @ w_group).astype(jnp.float32) + b_group.astype(jnp.float32)
    group_gate, group_idx = lax.top_k(jax.nn.softmax(group_logits, axis=-1), 1)
    expert_logits = ((ht @ w_expert).astype(jnp.float32) + b_expert.astype(jnp.float32)
                     ).reshape(n_tok, N_GROUPS, EXPERTS_PER_GROUP)
    sel = jnp.broadcast_to(group_idx[:, :, None], (n_tok, 1, EXPERTS_PER_GROUP))
    in_group = jnp.take_along_axis(expert_logits, sel, axis=1)[:, 0]
    top_p, top_i = lax.top_k(jax.nn.softmax(in_group, axis=-1), TOP_K)
    top_p = top_p / jnp.sum(top_p, axis=-1, keepdims=True)
    weights = group_gate * top_p
    expert_idx = group_idx * EXPERTS_PER_GROUP + top_i
    y = grouped_expert_mlp(ht, expert_idx, weights, w_gate, w_up, w_down)
    return y.reshape(bsz, seqlen, d)


def setup_inputs(seed: int = 0) -> dict:
    key = jax.random.key(seed)
    ks = jax.random.split(key, 32)
    f32 = jnp.float32
    d = D_MODEL

    def nrm(k, shape, fan_in, gain=1.0):
        return jax.random.normal(k, shape, f32) * (gain * fan_in ** -0.5)

    def gain_init(k, shape):
        return 1.0 + 0.02 * jax.random.normal(k, shape, f32)

    dt = jnp.exp(jax.random.uniform(ks[9], (N_A_LAYERS, GDN_HEADS), f32, math.log(1e-3), math.log(1e-1)))
    return {
        'x': jax.random.normal(ks[0], (BATCH, SEQ, d), f32),
        'c': jax.random.normal(ks[1], (BATCH, d), f32),
        'mod_w': nrm(ks[2], (DEPTH, d, 6 * d), d, 0.5),
        'mod_b': 0.02 * jax.random.normal(ks[3], (DEPTH, 6 * d), f32),
        'norm_mix_g': gain_init(ks[4], (DEPTH, d)),
        'norm_ffn_g': gain_init(ks[5], (DEPTH, d)),
        'gdn_w_in': nrm(ks[6], (N_A_LAYERS, d, GDN_PROJ), d),
        'gdn_conv_w': nrm(ks[7], (N_A_LAYERS, CONV_WIDTH, 3 * GDN_INNER), CONV_WIDTH),
        'gdn_a_log': jnp.log(jax.random.uniform(ks[8], (N_A_LAYERS, GDN_HEADS), f32, 1.0, 16.0)),
        'gdn_dt_bias': dt + jnp.log(-jnp.expm1(-dt)),
        'gdn_out_norm_g': gain_init(ks[10], (N_A_LAYERS, GDN_HEAD_DIM)),
        'gdn_w_out': nrm(ks[11], (N_A_LAYERS, GDN_INNER, d), GDN_INNER),
        'kv_mod_w': nrm(ks[12], (d, 2 * d), d, 0.5),
        'kv_mod_b': 0.02 * jax.random.normal(ks[13], (2 * d,), f32),
        'kv_norm_g': gain_init(ks[14], (d,)),
        'kv_w': nrm(ks[15], (d, 2 * FOX_INNER + FOX_HEADS), d),
        'kv_forget_b': jax.random.uniform(ks[16], (FOX_HEADS,), f32, 1.0, 5.0),
        'k_norm_g': gain_init(ks[17], (FOX_HEAD_DIM,)),
        'fox_w_qz': nrm(ks[18], (N_B_LAYERS, d, 2 * FOX_INNER), d),
        'fox_q_norm_g': gain_init(ks[19], (N_B_LAYERS, FOX_HEAD_DIM)),
        'fox_w_out': nrm(ks[20], (N_B_LAYERS, FOX_INNER, d), FOX_INNER),
        'moe_w_group': nrm(ks[21], (DEPTH, d, N_GROUPS), d),
        'moe_b_group': 0.01 * jax.random.normal(ks[22], (DEPTH, N_GROUPS), f32),
        'moe_w_expert': nrm(ks[23], (DEPTH, d, N_EXPERTS), d),
        'moe_b_expert': 0.01 * jax.random.normal(ks[24], (DEPTH, N_EXPERTS), f32),
        'moe_w_gate': nrm(ks[25], (DEPTH, N_EXPERTS, d, D_EXPERT), d),
        'moe_w_up': nrm(ks[26], (DEPTH, N_EXPERTS, d, D_EXPERT), d),
        'moe_w_down': nrm(ks[27], (DEPTH, N_EXPERTS, D_EXPERT, d), D_EXPERT),
    }


def reference(x, c, mod_w, mod_b, norm_mix_g, norm_ffn_g, gdn_w_in, gdn_conv_w, gdn_a_log, gdn_dt_bias,
              gdn_out_norm_g, gdn_w_out, kv_mod_w, kv_mod_b, kv_norm_g, kv_w, kv_forget_b, k_norm_g,
              fox_w_qz, fox_q_norm_g, fox_w_out, moe_w_group, moe_b_group, moe_w_expert, moe_b_expert,
              moe_w_gate, moe_w_up, moe_w_down):
    c_act = jax.nn.silu(c)
    k_sh = v_sh = fcum = None
    for layer in range(DEPTH):
        mod = c_act @ mod_w[layer] + mod_b[layer]
        sh1, sc1, gt1, sh2, sc2, gt2 = [m[:, None, :] for m in jnp.split(mod, 6, axis=-1)]
        h = modulate(x, norm_mix_g[layer], sh1, sc1)
        if layer < N_A_LAYERS:
            y = gdn_mixer(h, gdn_w_in[layer], gdn_conv_w[layer], gdn_a_log[layer], gdn_dt_bias[layer],
                          gdn_out_norm_g[layer], gdn_w_out[layer])
        else:
            j = layer - N_A_LAYERS
            y = fox_mixer(h, fox_w_qz[j], fox_q_norm_g[j], fox_w_out[j], k_sh, v_sh, fcum)
        x = x + gt1 * y
        h = modulate(x, norm_ffn_g[layer], sh2, sc2)
        x = x + gt2 * hier_moe(h, moe_w_group[layer], moe_b_group[layer], moe_w_expert[layer],
                               moe_b_expert[layer], moe_w_gate[layer], moe_w_up[layer], moe_w_down[layer])
        if layer == N_A_LAYERS - 1:
            k_sh, v_sh, fcum = shared_kv(x, c_act, kv_mod_w, kv_mod_b, kv_norm_g, kv_w, kv_forget_b, k_norm_g)
    return x
```

```python
import os
import numpy as np
import concourse.bass as bass
import concourse.mybir as mybir
from concourse.bass_utils import run_bass_kernel_spmd

F32 = mybir.dt.float32
BF16 = mybir.dt.bfloat16
I32 = mybir.dt.int32
U32 = mybir.dt.uint32
AF = mybir.ActivationFunctionType
ALU = mybir.AluOpType
AX = mybir.AxisListType

S = 4096
D = 1024
NT = S // 128
DEPTH = 4
EPS = 1e-6
GH = 8
GPROJ = 4112
FH = 16
NE = 32
DE = 512
NSLOT = 8
CAP = NSLOT * 128
NEG = -30000.0


class Buf:
    __slots__ = ("name", "last_w", "readers")

    def __init__(self, name=""):
        self.name = name
        self.last_w = None
        self.readers = {}


class Prog:
    ENGS = ("pe", "act", "dve", "pool", "sp")

    def __init__(self, nc, n_dma_sems=14):
        self.nc = nc
        self.streams = {e: [] for e in self.ENGS}
        self.sems = {}
        self.tick = {e: 0 for e in self.ENGS}
        self.waited = {e: {} for e in self.ENGS}
        self._ctx = []
        self.rec = None
        for e in self.ENGS:
            self._newsem("eng_" + e)
        self.dma_slots = {}
        self.n_dma_sems = n_dma_sems
        for q in ("sp", "act", "pool"):
            self.dma_slots[q] = {"next": 0, "count": [0] * n_dma_sems}
            for i in range(n_dma_sems):
                self._newsem("dma_%s_%d" % (q, i))

    def _newsem(self, key):
        cm = self.nc.semaphore(key)
        h = cm.__enter__()
        self._ctx.append(cm)
        self.sems[key] = h

    def close(self):
        for cm in reversed(self._ctx):
            cm.__exit__(None, None, None)

    def _collect(self, eng, reads, writes, is_dma=False):
        deps = {}

        def add(d):
            if deps.get(d[0], -1) < d[1]:
                deps[d[0]] = d[1]
        for b in reads:
            lw = b.last_w
            if lw is not None and (is_dma or not (lw[2] == eng and eng == "pe")):
                add(lw)
        for b in writes:
            if b.last_w is not None and (is_dma or b.last_w[2] != eng):
                add(b.last_w)
            for re_, d in b.readers.items():
                if is_dma or re_ != eng:
                    add(d)
        waits = []
        wd = self.waited[eng]
        for k, v in deps.items():
            if wd.get(k, -1) < v:
                wd[k] = v
                waits.append((k, v))
        return waits

    def _commit(self, eng, reads, writes, dep):
        for b in reads:
            b.readers[eng] = dep
        for b in writes:
            b.last_w = (dep[0], dep[1], eng)
            b.readers = {}

    def op(self, eng, fn, reads=(), writes=()):
        if self.rec is not None:
            self.rec.append(("op", eng, fn, tuple(reads), tuple(writes)))
            return
        waits = self._collect(eng, reads, writes)
        self.tick[eng] += 1
        key = "eng_" + eng
        self.streams[eng].append((waits, fn, key, 1))
        self._commit(eng, reads, writes, (key, self.tick[eng]))

    def dma(self, q, fn, reads=(), writes=()):
        if self.rec is not None:
            self.rec.append(("dma", q, fn, tuple(reads), tuple(writes)))
            return
        waits = self._collect(q, reads, writes, True)
        st = self.dma_slots[q]
        s = st["next"]
        st["next"] = (s + 1) % self.n_dma_sems
        key = "dma_%s_%d" % (q, s)
        prev = st["count"][s] * 16
        if prev > 0 and self.waited[q].get(key, -1) < prev:
            self.waited[q][key] = prev
            waits.append((key, prev))
        st["count"][s] += 1
        self.streams[q].append((waits, fn, key, 16))
        self._commit("dma_" + q + str(s), reads, writes, (key, st["count"][s] * 16))

    def replay_interleaved(self, lists, skew=4):
        units = []
        for lst in lists:
            u = []
            for item in lst:
                if item[0] == "op" and item[1] == "pe" and u and u[-1][-1][0] == "op" and u[-1][-1][1] == "pe":
                    u[-1].append(item)
                else:
                    u.append([item])
            units.append(u)
        pos = [0] * len(units)
        step = 0
        while any(pos[j] < len(units[j]) for j in range(len(units))):
            for j in range(len(units)):
                if step >= j * skew and pos[j] < len(units[j]):
                    for kind, a, fn, r, w in units[j][pos[j]]:
                        (self.op if kind == "op" else self.dma)(a, fn, r, w)
                    pos[j] += 1
            step += 1

    def begin_guard(self, cond):
        self._g_start = {e: len(self.streams[e]) for e in self.ENGS}
        self._g_waited = {e: dict(self.waited[e]) for e in self.ENGS}
        self._g_tick = dict(self.tick)
        self._g_dma = {q: list(st["count"]) for q, st in self.dma_slots.items()}
        self._g_cond = cond

    def end_guard(self):
        for e in self.ENGS:
            body = self.streams[e][self._g_start[e]:]
            del self.streams[e][self._g_start[e]:]
            if not body:
                continue
            incs = {}
            for waits, fn, key, inc in body:
                if fn is not None:
                    incs[key] = incs.get(key, 0) + inc
            base = {}
            for key in incs:
                if key.startswith("eng_"):
                    base[key] = self._g_tick[key[4:]]
                else:
                    _, q, sl = key.split("_")
                    base[key] = self._g_dma[q][int(sl)] * 16
            self.streams[e].append(("guard", self._g_cond, body, [(k, base[k], incs[k]) for k in incs]))
            self.waited[e] = self._g_waited[e]

    def barrier(self):
        targets = []
        for e in self.ENGS:
            if self.tick[e] > 0:
                targets.append(("eng_" + e, self.tick[e]))
        for q, st in self.dma_slots.items():
            for s, cnt in enumerate(st["count"]):
                if cnt > 0:
                    targets.append(("dma_%s_%d" % (q, s), cnt * 16))
        for e in self.ENGS:
            waits = []
            for k, v in targets:
                if k == "eng_" + e:
                    continue
                if self.waited[e].get(k, -1) < v:
                    self.waited[e][k] = v
                    waits.append((k, v))
            if waits:
                self.streams[e].append((waits, None, None, 0))

    def check(self):
        flat = {}
        for e in self.ENGS:
            out = []

            def walk(lst):
                for item in lst:
                    if item[0] == "guard":
                        walk(item[2])
                    else:
                        out.append(item)
            walk(self.streams[e])
            flat[e] = out
        sem = {k: 0 for k in self.sems}
        pos = {e: 0 for e in self.ENGS}
        progress = True
        while progress:
            progress = False
            for e in self.ENGS:
                lst = flat[e]
                while pos[e] < len(lst):
                    waits, fn, key, inc = lst[pos[e]]
                    if any(sem[k] < v for k, v in waits):
                        break
                    if fn is not None:
                        sem[key] += inc
                    pos[e] += 1
                    progress = True
        stuck = {e: (pos[e], len(flat[e])) for e in self.ENGS if pos[e] < len(flat[e])}
        if stuck:
            for e in stuck:
                waits = flat[e][pos[e]][0]
                print("STUCK", e, pos[e], [(k, v, sem[k]) for k, v in waits if sem[k] < v])
            raise RuntimeError("semaphore deadlock in generated program: %s" % stuck)

    def emit(self):
        nc = self.nc
        sems = self.sems
        streams = self.streams

        def run(engobj, lst):
            for item in lst:
                if item[0] == "guard":
                    _, cond, body, incs = item
                    with cond(engobj):
                        run(engobj, body)
                    with engobj.Else():
                        for k, basev, tot in incs:
                            if basev > 0:
                                engobj.wait_ge(sems[k], basev)
                            engobj.sem_inc(sems[k], tot)
                    continue
                waits, fn, key, inc = item
                for k, v in waits:
                    engobj.wait_ge(sems[k], v)
                if fn is not None:
                    fn(engobj).then_inc(sems[key], inc)

        with nc.Block() as block:
            @block.tensor
            def _(e):
                run(e, streams["pe"])

            @block.scalar
            def _(e):
                run(e, streams["act"])

            @block.vector
            def _(e):
                run(e, streams["dve"])

            @block.gpsimd
            def _(e):
                run(e, streams["pool"])

            @block.sync
            def _(e):
                run(e, streams["sp"])


class KB:
    def __init__(self, dbg=()):
        self.dbg = set(dbg)
        self.nc = bass.Bass("TRN2", target_bir_lowering=False)
        self.P = Prog(self.nc)
        self.scopes = [[]]
        self.uid = 0
        self.consts_ready = False

    def _nm(self, base):
        self.uid += 1
        return "%s_%d" % (base, self.uid)

    def sb(self, shape, dt, name="t"):
        cm = self.nc.sbuf_tensor(self._nm(name), list(shape), dt)
        t = cm.__enter__()
        self.scopes[-1].append(cm)
        return t

    def ps(self, shape, dt=F32, name="p"):
        cm = self.nc.psum_tensor(self._nm(name), list(shape), dt)
        t = cm.__enter__()
        self.scopes[-1].append(cm)
        return t

    def push(self):
        self.scopes.append([])

    def pop(self):
        self.P.barrier()
        for cm in reversed(self.scopes.pop()):
            cm.__exit__(None, None, None)

    def din(self, name, shape, dt=F32):
        return self.nc.dram_tensor(name, list(shape), dt, kind="ExternalInput").ap()

    def dout(self, name, shape, dt=F32):
        return self.nc.dram_tensor(name, list(shape), dt, kind="ExternalOutput").ap()

    def dscr(self, name, shape, dt=F32):
        if name in self.dbg:
            return self.nc.dram_tensor(name, list(shape), dt, kind="ExternalOutput").ap()
        return self.nc.dram_tensor(name, list(shape), dt, kind="Internal").ap()

    def op(self, eng, fn, r=(), w=()):
        self.P.op(eng, fn, r, w)

    def dma(self, out, in_, r=(), w=(), q="sp", **kw):
        self.P.dma(q, lambda e: e.dma_start(out=out, in_=in_, **kw), r, w)

    def mm(self, out, lhsT, rhs, start, stop, r=(), w=()):
        self.P.op("pe", lambda e: e.matmul(out, lhsT=lhsT, rhs=rhs, start=start, stop=stop), r, w)

    def tr(self, out, in_, ident, r=(), w=()):
        self.P.op("pe", lambda e: e.transpose(out=out, in_=in_, identity=ident), r, w)

    def act(self, out, in_, func, r=(), w=(), **kw):
        self.P.op("act", lambda e: e.activation(out=out, in_=in_, func=func, **kw), r, w)

    def tt(self, eng, out, in0, in1, op, r=(), w=()):
        self.P.op(eng, lambda e: e.tensor_tensor(out=out, in0=in0, in1=in1, op=op), r, w)

    def ts(self, eng, out, in0, s1, s2, op0, op1=None, r=(), w=()):
        if op1 is None:
            self.P.op(eng, lambda e: e.tensor_scalar(out=out, in0=in0, scalar1=s1, scalar2=None, op0=op0), r, w)
        else:
            self.P.op(eng, lambda e: e.tensor_scalar(out=out, in0=in0, scalar1=s1, scalar2=s2, op0=op0, op1=op1), r, w)

    def stt(self, eng, out, in0, scalar, in1, op0, op1, r=(), w=()):
        self.P.op(eng, lambda e: e.scalar_tensor_tensor(out=out, in0=in0, scalar=scalar, in1=in1, op0=op0, op1=op1), r, w)

    def copy(self, eng, out, in_, r=(), w=()):
        if eng == "act":
            self.P.op("act", lambda e: e.copy(out=out, in_=in_), r, w)
        else:
            self.P.op(eng, lambda e: e.tensor_copy(out=out, in_=in_), r, w)

    def memset(self, eng, ap, val, w=()):
        self.P.op(eng, lambda e: e.memset(ap, val), (), w)

    def make_consts(self):
        c = {}
        bc = Buf("consts")
        self.bc = bc
        idf = self.sb([128, 128], F32, "identf")
        self.memset("pool", idf[:], 0.0, [bc])
        self.op("pool", lambda e: e.affine_select(out=idf[:], in_=idf[:], pattern=[[-1, 128]], compare_op=ALU.not_equal,
                                                    fill=1.0, base=0, channel_multiplier=1), [bc], [bc])
        idb = self.sb([128, 128], BF16, "identb")
        self.copy("pool", idb[:], idf[:], [bc], [bc])
        onesf = self.sb([128, 128], F32, "onesf")
        self.memset("pool", onesf[:], 1.0, [bc])
        onesb = self.sb([128, 128], BF16, "onesb")
        self.memset("pool", onesb[:], 1.0, [bc])
        utri = self.sb([128, 128], F32, "utri")
        self.memset("pool", utri[:], 1.0, [bc])
        self.op("pool", lambda e: e.affine_select(out=utri[:], in_=utri[:], pattern=[[1, 128]], compare_op=ALU.is_ge,
                                                    fill=0.0, base=0, channel_multiplier=-1), [bc], [bc])
        sup = self.sb([128, 128], F32, "sup")
        self.memset("pool", sup[:], 1.0, [bc])
        self.op("pool", lambda e: e.affine_select(out=sup[:], in_=sup[:], pattern=[[1, 128]], compare_op=ALU.is_gt,
                                                    fill=0.0, base=0, channel_multiplier=-1), [bc], [bc])
        negm = self.sb([128, 128], F32, "negm")
        self.memset("pool", negm[:], 0.0, [bc])
        self.op("pool", lambda e: e.affine_select(out=negm[:], in_=negm[:], pattern=[[1, 128]], compare_op=ALU.is_ge,
                                                    fill=NEG, base=0, channel_multiplier=-1), [bc], [bc])
        negmb = self.sb([128, 128], BF16, "negmb")
        self.copy("pool", negmb[:], negm[:], [bc], [bc])
        epsc = self.sb([128, 1], F32, "epsc")
        self.memset("pool", epsc[:], EPS, [bc])
        onec = self.sb([128, 1], F32, "onec")
        self.memset("pool", onec[:], 1.0, [bc])
        c.update(idf=idf, idb=idb, onesf=onesf, onesb=onesb, utri=utri, sup=sup, negm=negm, negmb=negmb, epsc=epsc, onec=onec)
        self.c = c


def _silu_from(kb, out, in_, tmp, r, w, eng="dve"):
    kb.act(tmp, in_, AF.Sigmoid, r, w)
    kb.tt(eng, out, in_, tmp, ALU.mult, r, w)


def phase_mod(kb, T):
    c = kb.c
    kb.push()
    csb = kb.sb([128, 8], F32, "csb")
    cact = kb.sb([128, 8], F32, "cact")
    bcs = Buf()
    kb.dma(csb[:], T["c"].rearrange("o (p j) -> p (o j)", j=8), w=[bcs])
    kb.act(cact[:], csb[:], AF.Silu, [bcs], [bcs])
    NM = 6144 * 4 + 2048
    bmb = [Buf() for _ in range(3)]
    bmr = [Buf() for _ in range(3)]
    GW = 1024
    mw = [kb.sb([128, 8, GW], F32, "mw") for _ in range(3)]
    bmw = [Buf() for _ in range(3)]
    modb = [kb.sb([1, GW], F32, "modb") for _ in range(3)]
    modr = [kb.sb([1, GW], F32, "modr") for _ in range(3)]
    pm = [kb.ps([1, 512], F32, "pm") for _ in range(2)]
    bpm = [Buf(), Buf()]
    groups = []
    for l in range(4):
        wv = T["mod_w"][l].rearrange("(p j) n -> p j n", j=8)
        for n in range(6144 // GW):
            groups.append((wv[:, :, n * GW:(n + 1) * GW], T["mod_b"][l:l + 1, n * GW:(n + 1) * GW], l * 6144 + n * GW))
    wv = T["kv_mod_w"].rearrange("(p j) n -> p j n", j=8)
    for n in range(2048 // GW):
        groups.append((wv[:, :, n * GW:(n + 1) * GW], T["kv_mod_b"][0:1, n * GW:(n + 1) * GW], 24576 + n * GW))
    nm = 0
    for gi, (src, bsrc, col) in enumerate(groups):
        k3 = gi % 3
        kb.dma(mw[k3][:], src, w=[bmw[k3]])
        kb.dma(modb[k3][:], bsrc, w=[bmb[k3]])
        for hh in range(GW // 512):
            k2 = nm % 2
            nm += 1
            for j in range(8):
                kb.mm(pm[k2][:], cact[:, j:j + 1], mw[k3][:, j, hh * 512:(hh + 1) * 512], j == 0, j == 7, [bcs, bmw[k3]], [bpm[k2]])
            kb.tt("dve", modr[k3][:, hh * 512:(hh + 1) * 512], pm[k2][:], modb[k3][:, hh * 512:(hh + 1) * 512], ALU.add,
                  [bpm[k2], bmb[k3]], [bmr[k3]])
        kb.dma(T["modd"][0:1, col:col + GW], modr[k3][:], r=[bmr[k3]], w=[T["b_modd"]])
    kb.pop()


def load_mod(kb, T, layer, which):
    A = kb.sb([128, D], F32, "modA")
    Bt = kb.sb([128, D], F32, "modB")
    G = None
    gn = kb.sb([128, D], F32, "modg")
    b = Buf()
    if which == "kv":
        base = 24576
        gsrc = T["kv_norm_g"]
    else:
        base = layer * 6144 + (0 if which == "mix" else 3072)
        gsrc = T["norm_mix_g" if which == "mix" else "norm_ffn_g"][layer:layer + 1, :]
    md = T["modd"]
    kb.dma(Bt[:], md[0:1, base:base + 1024].partition_broadcast(128), r=[T["b_modd"]], w=[b])
    kb.dma(A[:], md[0:1, base + 1024:base + 2048].partition_broadcast(128), r=[T["b_modd"]], w=[b])
    kb.dma(gn[:], gsrc.partition_broadcast(128), w=[b])
    kb.stt("dve", A[:], A[:], 1.0, gn[:], ALU.add, ALU.mult, [b], [b])
    return A, Bt, G, b


class NormT:
    def __init__(self, kb, A, Bt, bmod, want32=False):
        self.kb = kb
        self.A, self.Bt, self.bmod = A, Bt, bmod
        self.xt = [kb.sb([128, D], F32, "xt") for _ in range(2)]
        self.ht = [kb.sb([128, D], F32, "ht") for _ in range(2)]
        self.junk = kb.sb([128, D], F32, "junk")
        self.ss = [kb.sb([128, 1], F32, "ss") for _ in range(2)]
        self.rs = [kb.sb([128, 1], F32, "rs") for _ in range(2)]
        self.want32 = want32
        self.pT = [kb.ps([128, 4, 128], F32, "pT") for _ in range(2)]
        if not want32:
            self.htb = [kb.sb([128, D], BF16, "htb") for _ in range(2)]
            self.pTb = [self.pT[0][:].bitcast(BF16), self.pT[1][:].bitcast(BF16)]
        self.b_xt = [Buf(), Buf()]
        self.b_ht = [Buf(), Buf()]
        self.b_junk = Buf()
        self.b_s = [Buf(), Buf()]
        self.b_pT = [Buf(), Buf()]
        self.n = 0

    def run(self, xsrc, bx, hT_dst, b_hT, hT32_dst=None, keep_x=False):
        st = self.run_a(xsrc, bx)
        return self.run_b(st, hT_dst, b_hT, hT32_dst)

    def run_a(self, xsrc, bx):
        kb = self.kb
        c = kb.c
        k = self.n % 2
        self.n += 1
        xt, ht = self.xt[k], self.ht[k]
        kb.dma(xt[:], xsrc, r=[bx], w=[self.b_xt[k]])
        kb.act(self.junk[:], xt[:], AF.Square, [self.b_xt[k]], [self.b_junk, self.b_s[k]], accum_out=self.ss[k][:])
        kb.act(self.rs[k][:], self.ss[k][:], AF.Sqrt, [self.b_s[k], kb.bc], [self.b_s[k]], scale=1.0 / D, bias=c["epsc"][:])
        kb.op("dve", lambda e: e.reciprocal(out=self.rs[k][:], in_=self.rs[k][:]), [self.b_s[k]], [self.b_s[k]])
        kb.stt("dve", ht[:], xt[:], self.rs[k][:, 0:1], self.A[:], ALU.mult, ALU.mult, [self.b_xt[k], self.b_s[k], self.bmod], [self.b_ht[k]])
        if self.want32:
            kb.tt("dve", ht[:], ht[:], self.Bt[:], ALU.add, [self.b_ht[k], self.bmod], [self.b_ht[k]])
        else:
            kb.tt("dve", self.htb[k][:], ht[:], self.Bt[:], ALU.add, [self.b_ht[k], self.bmod], [self.b_ht[k]])
        self.last_h = (ht, self.b_ht[k])
        return k

    def run_b(self, k, hT_dst, b_hT, hT32_dst=None):
        kb = self.kb
        c = kb.c
        xt, ht = self.xt[k], self.ht[k]
        if not self.want32:
            pv = self.pTb[0].rearrange("p a (b s) -> p (a b) s", s=128)
            for ch in range(8):
                kb.tr(pv[:, ch, :], self.htb[k][:, ch * 128:(ch + 1) * 128], c["idb"][:], [self.b_ht[k], kb.bc], [self.b_pT[0]])
            kb.copy("act", hT_dst[:, 0:4, :], pv[:, 0:4, :], [self.b_pT[0]], [b_hT])
            kb.copy("dve", hT_dst[:, 4:8, :], pv[:, 4:8, :], [self.b_pT[0]], [b_hT])
            return xt, self.b_xt[k]
        for half in range(2):
            for cc in range(4):
                ch = half * 4 + cc
                kb.tr(self.pT[half][:, cc, :], ht[:, ch * 128:(ch + 1) * 128], c["idf"][:], [self.b_ht[k], kb.bc], [self.b_pT[half]])
            eng = "act" if half == 0 else "dve"
            if hT_dst is not None:
                kb.copy(eng, hT_dst[:, half * 4:(half + 1) * 4, :], self.pT[half][:], [self.b_pT[half]], [b_hT])
            if hT32_dst is not None:
                eng2 = "dve" if half == 0 else "act"
                kb.copy(eng2, hT32_dst[:, half * 4:(half + 1) * 4, :], self.pT[half][:], [self.b_pT[half]], [b_hT])
        return xt, self.b_xt[k]


def pipeline(gens, extra=None):
    if os.environ.get("PIPE_SEQ"):
        for r, gf in enumerate(gens):
            if extra is not None:
                extra(r)
            for _ in gf():
                pass
        return
    live = []
    gi = 0
    r = 0
    while gi < len(gens) or live:
        if extra is not None:
            extra(r)
        nxt = []
        for g_ in live:
            try:
                next(g_)
                nxt.append(g_)
            except StopIteration:
                pass
        live = nxt
        if gi < len(gens):
            g_ = gens[gi]()
            gi += 1
            try:
                next(g_)
                live.append(g_)
            except StopIteration:
                pass
        r += 1


def load_w_bf16(kb, dst, src, K, N, b, kchunks=None):
    nk = K // 128
    for c in range(nk):
        n0 = 0
        while n0 < N:
            n1 = min(N, n0 + 2048)
            kb.dma(dst[:, c, n0:n1], src[c * 128:(c + 1) * 128, n0:n1], w=[b], q="pool")
            n0 = n1


def phase_gdn_proj(kb, T, l, xsrc, bx):
    c = kb.c
    kb.push()
    A, Bt, G, bmod = load_mod(kb, T, l, "mix")
    Win = kb.sb([128, 8, GPROJ], BF16, "Win")
    bW = Buf()
    load_w_bf16(kb, Win, T["gdn_w_in"][l], D, GPROJ, bW)
    cwr = kb.sb([96, 128], F32, "cwr")
    bcw = Buf()
    kb.dma(cwr[:], T["gdn_conv_w"][l].rearrange("k (c p) -> (k c) p", p=128), w=[bcw])
    cw = kb.sb([128, 96], F32, "cw")
    nega = kb.sb([128, 8], F32, "nega")
    dtb = kb.sb([128, 8], F32, "dtb")
    bsm = Buf()
    kb.dma(nega[:], T["gdn_a_log"][l:l + 1, :].partition_broadcast(128), w=[bsm])
    kb.dma(dtb[:], T["gdn_dt_bias"][l:l + 1, :].partition_broadcast(128), w=[bsm])
    kb.act(nega[:], nega[:], AF.Exp, [bsm], [bsm])
    kb.ts("dve", nega[:], nega[:], -1.0, None, ALU.mult, None, [bsm], [bsm])

    nt = NormT(kb, A, Bt, bmod)
    hTg = [kb.sb([128, 8, 512], BF16, "hTg") for _ in range(2)]
    b_hTg = [Buf(), Buf()]
    halo = kb.sb([128, 24, 4], BF16, "halo")
    b_halo = Buf()
    kb.memset("dve", halo[:], 0.0, [b_halo])
    pre = [kb.sb([128, 516], BF16, "pre") for _ in range(2)]
    b_pre = [Buf(), Buf()]
    dg = [kb.sb([128, 4, 128], BF16, "dg") for _ in range(2)]
    b_dg = [Buf(), Buf()]
    pq = [kb.ps([128, 512], F32, "pq") for _ in range(2)]
    b_pq = [Buf(), Buf()]
    psmall = kb.ps([128, 512], F32, "psmall")
    pcw = psmall[:, 32:128]
    kb.tr(pcw, cwr[:], c["idf"][0:96, 0:96], [bcw, kb.bc], [bcw])
    kb.copy("dve", cw[:], pcw, [bcw], [bcw])
    pc = kb.ps([128, 512], F32, "pc")
    b_pc = Buf()
    pn = kb.ps([128, 512], F32, "pn")
    b_pn = Buf()
    qst = [kb.sb([128, 4, 24, 128], BF16, "qst") for _ in range(2)]
    b_qst = [Buf(), Buf()]
    pab = psmall[:, 0:16]
    b_pab = Buf()
    zsg = [kb.sb([128, D], F32, "zsg") for _ in range(2)]
    zo = [kb.sb([128, D], BF16, "zo") for _ in range(2)]
    b_z = [Buf(), Buf()]
    ab = [kb.sb([128, 16], F32, "ab") for _ in range(2)]
    gbo = [kb.sb([128, 16], F32, "gbo") for _ in range(2)]
    b_ab = [Buf(), Buf()]
    pgc = psmall[:, 16:24]
    b_pgc = b_pab
    pc2 = [pc, kb.ps([128, 512], F32, "pc2")]
    b_pc2 = [b_pc, Buf()]
    sg3 = [kb.sb([128, 512], F32, "sg3") for _ in range(3)]
    qk3 = [kb.sb([128, 512], F32, "qk3") for _ in range(3)]
    sq3 = [kb.sb([128, 512], BF16, "sq3") for _ in range(3)]
    rn3 = [kb.sb([128, 512], F32, "rn3") for _ in range(3)]
    b_t3 = [Buf() for _ in range(3)]
    nz = 0

    def tile_small(g, t):
        nonlocal nz
        kg = g % 2
        i = g * 4 + t
        kz = nz % 2
        nz += 1
        for n in range(2):
            for cc in range(8):
                kb.mm(pq[n][:], hTg[kg][:, cc, t * 128:(t + 1) * 128], Win[:, cc, 3072 + n * 512:3072 + (n + 1) * 512],
                      cc == 0, cc == 7, [b_hTg[kg], bW], [b_pq[n]])
        for cc in range(8):
            kb.mm(pab, hTg[kg][:, cc, t * 128:(t + 1) * 128], Win[:, cc, 4096:4112], cc == 0, cc == 7, [b_hTg[kg], bW], [b_pab])
        for n in range(2):
            kb.act(zsg[kz][:, n * 512:(n + 1) * 512], pq[n][:], AF.Sigmoid, [b_pq[n]], [b_z[kz]])
            kb.tt("dve", zo[kz][:, n * 512:(n + 1) * 512], pq[n][:], zsg[kz][:, n * 512:(n + 1) * 512], ALU.mult, [b_pq[n], b_z[kz]], [b_z[kz]])
        kb.dma(T["sz_d"][i * 128:(i + 1) * 128, :], zo[kz][:], r=[b_z[kz]], w=[T["b_sz"][i]])
        kb.copy("dve", ab[kz][:], pab, [b_pab], [b_ab[kz]])
        kb.act(gbo[kz][:, 8:16], ab[kz][:, 8:16], AF.Sigmoid, [b_ab[kz]], [b_ab[kz]])
        kb.tt("dve", ab[kz][:, 0:8], ab[kz][:, 0:8], dtb[:], ALU.add, [b_ab[kz], bsm], [b_ab[kz]])
        kb.act(ab[kz][:, 0:8], ab[kz][:, 0:8], AF.Exp, [b_ab[kz]], [b_ab[kz]])
        kb.act(ab[kz][:, 0:8], ab[kz][:, 0:8], AF.Ln, [b_ab[kz], kb.bc], [b_ab[kz]], bias=c["onec"][:])
        kb.tt("dve", ab[kz][:, 0:8], ab[kz][:, 0:8], nega[:], ALU.mult, [b_ab[kz], bsm], [b_ab[kz]])
        kb.mm(pgc, c["utri"][:], ab[kz][:, 0:8], True, True, [b_ab[kz], kb.bc], [b_pgc])
        kb.copy("dve", gbo[kz][:, 0:8], pgc, [b_pgc], [b_ab[kz]])
        kb.dma(T["gb_d"][i * 128:(i + 1) * 128, :], gbo[kz][:], r=[b_ab[kz]], w=[T["b_gb"][i]])

    def chunk_gen(g, ch):
        kg = g % 2
        k2 = ch % 2
        k3 = ch % 3
        ov = qst[kg][:, :, ch, :]

        def gen():
            for cc in range(8):
                kb.mm(pq[k2][:], Win[:, cc, ch * 128:(ch + 1) * 128], hTg[kg][:, cc, :], cc == 0, cc == 7, [bW, b_hTg[kg]], [b_pq[k2]])
            yield
            kb.copy("dve", pre[k2][:, 0:4], halo[:, ch, :], [b_halo], [b_pre[k2]])
            kb.copy("act", pre[k2][:, 4:516], pq[k2][:], [b_pq[k2]], [b_pre[k2]])
            kb.copy("dve", halo[:, ch, :], pre[k2][:, 512:516], [b_pre[k2]], [b_halo])
            for kk in range(4):
                col = kk * 24 + ch
                kb.ts("dve", dg[k2][:, kk, :], c["idb"][:], cw[:, col:col + 1], None, ALU.mult, None, [bcw, kb.bc], [b_dg[k2]])
            yield
            for kk in range(4):
                kb.mm(pc2[k2][:], dg[k2][:, kk, :], pre[k2][:, 1 + kk:513 + kk], kk == 0, kk == 3, [b_dg[k2], b_pre[k2]], [b_pc2[k2]])
            yield
            kb.act(sg3[k3][:], pc2[k2][:], AF.Sigmoid, [b_pc2[k2]], [b_t3[k3]])
            if ch >= 16:
                kb.tt("dve", ov, pc2[k2][:].rearrange("p (t s) -> p t s", t=4), sg3[k3][:].rearrange("p (t s) -> p t s", t=4), ALU.mult,
                      [b_pc2[k2], b_t3[k3]], [b_qst[kg]])
                return
            kb.tt("dve", qk3[k3][:], pc2[k2][:], sg3[k3][:], ALU.mult, [b_pc2[k2], b_t3[k3]], [b_t3[k3]])
            kb.tt("dve", sq3[k3][:], qk3[k3][:], qk3[k3][:], ALU.mult, [b_t3[k3]], [b_t3[k3]])
            yield
            kb.mm(pn[:], c["onesb"][:], sq3[k3][:], True, True, [b_t3[k3], kb.bc], [b_pn])
            yield
            kb.act(rn3[k3][:], pn[:], AF.Sqrt, [b_pn, kb.bc], [b_t3[k3]], bias=c["epsc"][:])
            kb.op("dve", lambda e: e.reciprocal(out=rn3[k3][:], in_=rn3[k3][:]), [b_t3[k3]], [b_t3[k3]])
            qs = (128.0 ** -0.5) if ch < 8 else 1.0
            kb.stt("dve", ov, qk3[k3][:].rearrange("p (t s) -> p t s", t=4), qs, rn3[k3][:].rearrange("p (t s) -> p t s", t=4),
                   ALU.mult, ALU.mult, [b_t3[k3]], [b_qst[kg]])
        return gen

    NG = S // 512
    for t in range(4):
        nt.run(xsrc[t * 128:(t + 1) * 128, :], bx[t], hTg[0][:, :, t * 128:(t + 1) * 128], b_hTg[0])
    for g in range(NG):
        kg = g % 2
        for t in range(4):
            tile_small(g, t)
        pend = {}

        def extra(r, g=g):
            if g + 1 >= NG:
                return
            t, ph = divmod(r, 6)
            if t < 4 and ph == 1:
                i = (g + 1) * 4 + t
                pend[t] = nt.run_a(xsrc[i * 128:(i + 1) * 128, :], bx[i])
            if t < 4 and ph == 4:
                kn = (g + 1) % 2
                nt.run_b(pend[t], hTg[kn][:, :, t * 128:(t + 1) * 128], b_hTg[kn])

        pipeline([chunk_gen(g, ch) for ch in range(24)], extra)
        for t in range(4):
            i = g * 4 + t
            kb.dma(T["qkv_d"][i], qst[kg][:, t, :, :].rearrange("p c s -> p (c s)"), r=[b_qst[kg]], w=[T["b_qkv"][i]])
    kb.pop()


SOLVE_DT = F32
F32R = mybir.dt.float32r
SOLVE_R = False


def b3(ap, shape):
    return ap.to_broadcast(shape)


def phase_gdn_delta(kb, T, l, xsrc, bx, xdst, bxd):
    c = kb.c
    SD = SOLVE_DT
    kb.push()
    H3 = [128, 8, 128]
    G = kb.sb([128, D], F32, "G")
    bG = Buf()
    base = l * 6144 + 2048
    kb.dma(G[:], T["modd"][0:1, base:base + 1024].partition_broadcast(128), r=[T["b_modd"]], w=[bG])
    gon = kb.sb([128, 128], F32, "gon")
    kb.dma(gon[:], T["gdn_out_norm_g"][l:l + 1, :].partition_broadcast(128), w=[bG])
    Wout = kb.sb([128, 8, D], BF16, "Wout")
    bW = Buf()
    load_w_bf16(kb, Wout, T["gdn_w_out"][l], D, D, bW)
    S32 = kb.sb(H3, F32, "S32")
    Sb = kb.sb(H3, BF16, "Sb")
    bS32 = Buf()
    bSb = Buf()
    kb.memset("dve", S32[:], 0.0, [bS32])
    kb.memset("dve", Sb[:], 0.0, [bSb])
    idf3 = c["idf"][:].unsqueeze(1).to_broadcast(H3)
    negm3 = c["negm"][:].unsqueeze(1).to_broadcast(H3)
    sup3 = c["sup"][:].unsqueeze(1).to_broadcast(H3)
    gon3 = gon[:].unsqueeze(1).to_broadcast(H3)

    R = [kb.ps(H3, F32, "R") for _ in range(3)]
    bR = [Buf() for _ in range(3)]
    Rb = kb.ps([128, 2, 8, 128], BF16, "Rb")
    bRb = [Buf(), Buf()]

    qkvt = [kb.sb([128, 24, 128], BF16, "qkvt") for _ in range(2)]
    b_qkvt = [Buf(), Buf()]
    gb = [kb.sb([128, 16], F32, "gb") for _ in range(2)]
    glast = [kb.sb([128, 8], F32, "glast") for _ in range(2)]
    b_gb = [Buf(), Buf()]
    szt = [kb.sb([128, D], BF16, "szt") for _ in range(2)]
    b_sz = [Buf(), Buf()]
    xt = [kb.sb([128, D], F32, "xt") for _ in range(2)]
    b_xt = [Buf(), Buf()]
    sc = kb.sb([128, 5, 8], F32, "sc")
    b_sc = Buf()

    def t3(dt, nm):
        return kb.sb(H3, dt, nm), Buf()
    kb_tm, b_kb = t3(BF16, "kb_tm")
    kbe_tm, b_kbe = t3(BF16, "kbe_tm")
    kdec_tm, b_kdec = t3(BF16, "kdec_tm")
    vb_tm, b_vb = t3(BF16, "vb_tm")
    diagc, b_diag = t3(F32, "diagc")
    tmpf, b_tmpf = t3(F32, "tmpf")
    DT, b_DT = t3(F32, "DT")
    kbT, b_kbT = t3(BF16, "kbT")
    SDA = F32R if (SOLVE_R and SD == F32) else SD
    AT, b_AT = t3(SDA, "AT")
    Am, b_A = t3(SDA, "Am")
    Mp = [t3(SDA, "Mp") for _ in range(2)]
    MTp = [t3(SDA, "MTp") for _ in range(2)]
    Xp = [t3(SDA, "Xp") for _ in range(2)]
    Xb, b_Xb = t3(BF16, "Xb")
    wT, b_wT = t3(BF16, "wT")
    u, b_u = t3(F32, "u")
    vnew, b_vnew = t3(BF16, "vnew")
    attnT, b_attn = t3(BF16, "attnT")
    tmpo, b_tmpo = t3(F32, "tmpo")
    o, b_o = t3(F32, "o")
    sqo, b_sqo = t3(F32, "sqo")
    og = kb.sb([128, D], BF16, "og")
    b_og = Buf()
    ogT, b_ogT = t3(BF16, "ogT")
    ssq = kb.sb([128, 2, 8], F32, "ssq")
    b_ssq = Buf()
    yt = kb.sb([128, D], F32, "yt")
    b_yt = Buf()
    idS = c["idf"] if SD == F32 else c["idb"]

    def loads(i):
        k = i % 2
        kb.dma(qkvt[k][:].rearrange("p c s -> p (c s)"), T["qkv_d"][i], r=[T["b_qkv"][i]], w=[b_qkvt[k]])
        kb.dma(gb[k][:], T["gb_d"][i * 128:(i + 1) * 128, :], r=[T["b_gb"][i]], w=[b_gb[k]])
        kb.dma(glast[k][:], T["gb_d"][i * 128 + 127:i * 128 + 128, 0:8].partition_broadcast(128), r=[T["b_gb"][i]], w=[b_gb[k]])
        kb.dma(szt[k][:], T["sz_d"][i * 128:(i + 1) * 128, :], r=[T["b_sz"][i]], w=[b_sz[k]])
        kb.dma(xt[k][:], xsrc[i * 128:(i + 1) * 128, :], r=[bx[i]], w=[b_xt[k]])

    loads(0)
    for i in range(NT):
        k = i % 2
        if i + 1 < NT:
            loads(i + 1)
        q3 = qkvt[k][:, 0:8, :]
        k3 = qkvt[k][:, 8:16, :]
        v3 = qkvt[k][:, 16:24, :]
        gc = gb[k][:, 0:8]
        beta = gb[k][:, 8:16]
        egc, bege, dk, egl = sc[:, 0, :], sc[:, 1, :], sc[:, 2, :], sc[:, 3, :]
        rdeps = [b_gb[k]]
        kb.act(egc, gc, AF.Exp, rdeps, [b_sc])
        kb.tt("dve", bege, egc, beta, ALU.mult, rdeps + [b_sc], [b_sc])
        kb.tt("dve", dk, glast[k][:], gc, ALU.subtract, rdeps, [b_sc])
        kb.act(dk, dk, AF.Exp, [b_sc], [b_sc])
        kb.act(egl, glast[k][:], AF.Exp, rdeps, [b_sc])
        for h in range(8):
            kb.tr(Rb[:, 0, h, :], qkvt[k][:, 8 + h, :], c["idb"][:], [b_qkvt[k], kb.bc], [bRb[0]])
        for h in range(8):
            kb.tr(Rb[:, 1, h, :], qkvt[k][:, 16 + h, :], c["idb"][:], [b_qkvt[k], kb.bc], [bRb[1]])
        kb.tt("dve", kb_tm[:], Rb[:, 0, :, :], beta.unsqueeze(2).to_broadcast(H3), ALU.mult, [bRb[0], b_gb[k]], [b_kb])
        kb.tt("dve", kbe_tm[:], Rb[:, 0, :, :], bege.unsqueeze(2).to_broadcast(H3), ALU.mult, [bRb[0], b_sc], [b_kbe])
        kb.tt("dve", kdec_tm[:], Rb[:, 0, :, :], dk.unsqueeze(2).to_broadcast(H3), ALU.mult, [bRb[0], b_sc], [b_kdec])
        kb.tt("dve", vb_tm[:], Rb[:, 1, :, :], beta.unsqueeze(2).to_broadcast(H3), ALU.mult, [bRb[1], b_gb[k]], [b_vb])
        kb.tt("dve", diagc[:], idf3, gc.unsqueeze(2).to_broadcast(H3), ALU.mult, [kb.bc, b_gb[k]], [b_diag])
        for hh in range(2):
            kb.mm(R[0][:, hh * 4:(hh + 1) * 4, :], c["onesf"][:], diagc[:, hh * 4:(hh + 1) * 4, :], True, True, [kb.bc, b_diag], [bR[0]])
        kb.tt("dve", tmpf[:], R[0][:], gc.unsqueeze(2).to_broadcast(H3), ALU.subtract, [bR[0], b_gb[k]], [b_tmpf])
        kb.tt("dve", tmpf[:], tmpf[:], negm3, ALU.add, [b_tmpf, kb.bc], [b_tmpf])
        kb.act(DT[:], tmpf[:], AF.Exp, [b_tmpf], [b_DT])
        for h in range(8):
            kb.tr(Rb[:, 0, h, :], kb_tm[:, h, :], c["idb"][:], [b_kb, kb.bc], [bRb[0]])
        kb.copy("act", kbT[:], Rb[:, 0, :, :], [bRb[0]], [b_kbT])
        for h in range(8):
            kb.mm(R[1][:, h, :], qkvt[k][:, 8 + h, :], kbT[:, h, :], True, True, [b_qkvt[k], b_kbT], [bR[1]])
        kb.tt("dve", tmpf[:], R[1][:], DT[:], ALU.mult, [bR[1], b_DT], [b_tmpf])
        kb.tt("dve", AT[:], tmpf[:], sup3, ALU.mult, [b_tmpf, kb.bc], [b_AT])
        if SD == F32:
            for h in range(8):
                kb.tr(R[0][:, h, :], AT[:, h, :].bitcast(F32) if SDA == F32R else AT[:, h, :], idS[:], [b_AT, kb.bc], [bR[0]])
            kb.copy("act", Am[:], R[0][:], [bR[0]], [b_A])
        else:
            for h in range(8):
                kb.tr(Rb[:, 1, h, :], AT[:, h, :], idS[:], [b_AT, kb.bc], [bRb[1]])
            kb.copy("act", Am[:], Rb[:, 1, :, :], [bRb[1]], [b_A])
        X, bX = Xp[0]
        kb.tt("dve", X[:], idf3, AT[:], ALU.subtract, [kb.bc, b_AT], [bX])
        HS = [slice(0, 4), slice(4, 8)]
        hbR = [[Buf(), Buf()] for _ in range(3)]
        for r_ in range(3):
            for hf in range(2):
                hbR[r_][hf].last_w = bR[r_].last_w
                hbR[r_][hf].readers = dict(bR[r_].readers)
        hM = [Buf(), Buf()]
        hMT = [Buf(), Buf()]
        hX = [Buf(), Buf()]
        for hf in range(2):
            hM[hf].last_w = b_A.last_w
            hMT[hf].last_w = b_AT.last_w
            hX[hf].last_w = bX.last_w
        M, MT = Am, AT
        all_half_bufs = []
        for it in range(6):
            Mn, _bMn = Mp[it % 2]
            MTn, _bMTn = MTp[it % 2]
            Xn, _bXn = Xp[(it + 1) % 2]
            hMn = [Buf(), Buf()]
            hMTn = [Buf(), Buf()]
            hXn = [Buf(), Buf()]
            for hf in range(2):
                hMn[hf].readers = dict(_bMn.readers); hMn[hf].last_w = _bMn.last_w
                hMTn[hf].readers = dict(_bMTn.readers); hMTn[hf].last_w = _bMTn.last_w
                hXn[hf].readers = dict(_bXn.readers); hXn[hf].last_w = _bXn.last_w
            for hf in range(2):
                hs = HS[hf]
                for h in range(hs.start, hs.stop):
                    kb.mm(R[0][:, h, :], MT[:, h, :], M[:, h, :], True, True, [hMT[hf], hM[hf]], [hbR[0][hf]])
                if it < 5:
                    for h in range(hs.start, hs.stop):
                        kb.mm(R[1][:, h, :], M[:, h, :], MT[:, h, :], True, True, [hMT[hf], hM[hf]], [hbR[1][hf]])
                kb.copy("act", Mn[:, hs, :], R[0][:, hs, :], [hbR[0][hf]], [hMn[hf]])
                if it < 5:
                    kb.copy("dve", MTn[:, hs, :], R[1][:, hs, :], [hbR[1][hf]], [hMTn[hf]])
            for hf in range(2):
                hs = HS[hf]
                for h in range(hs.start, hs.stop):
                    kb.mm(R[2][:, h, :], Mn[:, h, :], X[:, h, :], True, True, [hMn[hf], hX[hf]], [hbR[2][hf]])
                kb.tt("dve", Xn[:, hs, :], R[2][:, hs, :], X[:, hs, :], ALU.add, [hbR[2][hf], hX[hf]], [hXn[hf]])
            for whole, halves in ((_bMn, hMn), (_bMTn, hMTn), (_bXn, hXn)):
                pass
            X = Xn
            M, MT = Mn, MTn
            hM, hMT, hX = hMn, hMTn, hXn
            all_half_bufs.append((hMn, hMTn, hXn))
        bX_halves = hX
        bR_half = hbR
        if SD == F32:
            kb.copy("dve", Xb[:], X[:], list(bX_halves), [b_Xb])
            XB, bXB = Xb, b_Xb
        else:
            XB, bXB = X, bX
        for h in range(8):
            kb.mm(R[0][:, h, :], kbe_tm[:, h, :], XB[:, h, :], True, True, [b_kbe, bXB], [bR[0]] + bR_half[0])
        for h in range(8):
            kb.mm(R[1][:, h, :], XB[:, h, :], vb_tm[:, h, :], True, True, [b_vb, bXB], [bR[1]] + bR_half[1])
        kb.copy("act", wT[:], R[0][:], [bR[0]], [b_wT])
        kb.copy("dve", u[:], R[1][:], [bR[1]], [b_u])
        for h in range(8):
            kb.mm(R[2][:, h, :], wT[:, h, :], Sb[:, h, :], True, True, [b_wT, bSb], [bR[2]] + bR_half[2])
        for h in range(8):
            kb.mm(R[0][:, h, :], qkvt[k][:, h, :], Sb[:, h, :], True, True, [b_qkvt[k], bSb], [bR[0]])
        for h in range(8):
            kb.mm(R[1][:, h, :], qkvt[k][:, 8 + h, :], qkvt[k][:, h, :], True, True, [b_qkvt[k]], [bR[1]])
        kb.tt("dve", vnew[:], u[:], R[2][:], ALU.subtract, [b_u, bR[2]], [b_vnew])
        kb.tt("dve", tmpo[:], R[0][:], egc.unsqueeze(2).to_broadcast(H3), ALU.mult, [bR[0], b_sc], [b_tmpo])
        kb.tt("dve", attnT[:], R[1][:], DT[:], ALU.mult, [bR[1], b_DT], [b_attn])
        for h in range(8):
            kb.mm(R[2][:, h, :], kdec_tm[:, h, :], vnew[:, h, :], True, True, [b_kdec, b_vnew], [bR[2]])
        for h in range(8):
            kb.mm(R[1][:, h, :], attnT[:, h, :], vnew[:, h, :], True, True, [b_attn, b_vnew], [bR[1]])
        kb.tt("dve", S32[:], S32[:], egl.unsqueeze(2).to_broadcast(H3), ALU.mult, [bS32, b_sc], [bS32])
        kb.tt("dve", S32[:], S32[:], R[2][:], ALU.add, [bS32, bR[2]], [bS32])
        kb.copy("act", Sb[:], S32[:], [bS32], [bSb])
        kb.tt("dve", o[:], tmpo[:], R[1][:], ALU.add, [b_tmpo, bR[1]], [b_o])
        kb.tt("dve", sqo[:], o[:], o[:], ALU.mult, [b_o], [b_sqo])
        kb.op("dve", lambda e: e.tensor_reduce(out=ssq[:, 0, :], in_=sqo[:], axis=AX.X, op=ALU.add), [b_sqo], [b_ssq])
        kb.act(ssq[:, 1, :], ssq[:, 0, :], AF.Sqrt, [b_ssq, kb.bc], [b_ssq], scale=1.0 / 128, bias=c["epsc"][:])
        kb.op("dve", lambda e: e.reciprocal(out=ssq[:, 1, :], in_=ssq[:, 1, :]), [b_ssq], [b_ssq])
        kb.tt("dve", sqo[:], o[:], ssq[:, 1, :].unsqueeze(2).to_broadcast(H3), ALU.mult, [b_o, b_ssq], [b_sqo])
        kb.tt("dve", sqo[:], sqo[:], gon3, ALU.mult, [b_sqo, bG], [b_sqo])
        kb.tt("dve", og[:], sqo[:].rearrange("p h d -> p (h d)"), szt[k][:], ALU.mult, [b_sqo, b_sz[k]], [b_og])
        for cc in range(8):
            kb.tr(Rb[:, 1, cc, :], og[:, cc * 128:(cc + 1) * 128], c["idb"][:], [b_og, kb.bc], [bRb[1]])
        kb.copy("act", ogT[:], Rb[:, 1, :, :], [bRb[1]], [b_ogT])
        R0f = R[0][:].rearrange("p h d -> p (h d)")
        for n in range(2):
            for cc in range(8):
                kb.mm(R0f[:, n * 512:(n + 1) * 512], ogT[:, cc, :], Wout[:, cc, n * 512:(n + 1) * 512], cc == 0, cc == 7, [b_ogT, bW], [bR[0]])
        kb.tt("dve", yt[:], R0f, G[:], ALU.mult, [bR[0], bG], [b_yt])
        kb.tt("dve", yt[:], yt[:], xt[k][:], ALU.add, [b_yt, b_xt[k]], [b_yt])
        kb.dma(xdst[i * 128:(i + 1) * 128, :], yt[:], r=[b_yt], w=[bxd[i]])
    kb.pop()


OOB_IDX = 1 << 28
_REGS = {}


def breg(e, val):
    key = (id(e), val)
    if key not in _REGS:
        _REGS[key] = e.to_reg(val)
    return _REGS[key]


def phase_moe(kb, T, l, xsrc, bx, xdst, bxd):
    c = kb.c
    kb.push()
    wts = kb.sb([128, NT, 2], F32, "wts")
    b_wts = Buf()
    kb.push()
    A, Bt, G_unused, bmod = load_mod(kb, T, l, "ffn")
    nt = NormT(kb, A, Bt, bmod, want32=True)
    Wr = kb.sb([128, 8, 36], F32, "Wr")
    bWr = Buf()
    kb.dma(Wr[:, :, 0:4], T["moe_w_group"][l].rearrange("(c p) n -> p c n", p=128), w=[bWr])
    kb.dma(Wr[:, :, 4:36], T["moe_w_expert"][l].rearrange("(c p) n -> p c n", p=128), w=[bWr])
    rb = kb.sb([128, 36], F32, "rb")
    kb.dma(rb[:, 0:4], T["moe_b_group"][l:l + 1, :].partition_broadcast(128), w=[bWr])
    kb.dma(rb[:, 4:36], T["moe_b_expert"][l:l + 1, :].partition_broadcast(128), w=[bWr])
    egrp = kb.sb([128, 32], F32, "egrp")
    eio = kb.sb([128, 32], F32, "eio")
    eioi = kb.sb([128, 32], I32, "eioi")
    bce = Buf()
    for g in range(4):
        kb.memset("dve", egrp[:, g * 8:(g + 1) * 8], float(g), [bce])
    kb.op("pool", lambda e: e.iota(eioi[:], pattern=[[1, 32]], base=0, channel_multiplier=0), (), [bce])
    kb.copy("dve", eio[:], eioi[:], [bce], [bce])
    sent = kb.sb([128, NE * CAP * 2 // 128], I32, "sent")
    kb.memset("dve", sent[:], OOB_IDX, [bce])
    kb.dma(T["rowasg"].rearrange("(p j) o -> p (j o)", p=128), sent[:], r=[bce], w=[T["b_rowasg"]])
    cnt = kb.sb([128, 32], F32, "cnt")
    b_cnt = Buf()
    kb.memset("dve", cnt[:], 0.0, [b_cnt])
    hT32 = [kb.sb([128, 8, 128], F32, "hT32") for _ in range(2)]
    b_hT32 = [Buf(), Buf()]
    hb = [kb.sb([128, D], BF16, "hb") for _ in range(2)]
    b_hb = [Buf(), Buf()]
    pl = kb.ps([128, 512], F32, "pl")
    b_pl = Buf()
    pp = kb.ps([128, 512], F32, "pp")
    b_pp = Buf()
    W = 160
    sm = [kb.sb([128, W], F32, "sm") for _ in range(2)]
    smu = [kb.sb([128, 16], U32, "smu") for _ in range(2)]
    smi = [kb.sb([128, 4], I32, "smi") for _ in range(2)]
    aid_all = kb.sb([128, NT, 2, 2], I32, "aid_all")
    kb.op("pool", lambda e: e.iota(aid_all[:], pattern=[[128, NT], [S, 2], [0, 2]], base=0, channel_multiplier=1), (), [bce])
    b_sm = [Buf(), Buf()]
    recs = []
    for i in range(NT):
        k = i % 2
        kb.P.rec = []
        nt.run(xsrc[i * 128:(i + 1) * 128, :], bx[i], None, b_hT32[k], hT32_dst=hT32[k])
        ht, b_ht = nt.last_h
        kb.copy("dve", hb[k][:], ht[:], [b_ht], [b_hb[k]])
        kb.dma(T["hd"][i * 128:(i + 1) * 128, :], hb[k][:], r=[b_hb[k]], w=[T["b_hd"]])
        kb.dma(T["hd"][S + i * 128:S + (i + 1) * 128, :], hb[k][:], r=[b_hb[k]], w=[T["b_hd"]])
        for cc in range(8):
            kb.mm(pl[:, 0:36], hT32[k][:, cc, :], Wr[:, cc, :], cc == 0, cc == 7, [b_hT32[k], bWr], [b_pl])
        m = sm[k]
        B = [b_sm[k]]
        lg8 = m[:, 0:8]
        le = m[:, 8:40]
        gmax = m[:, 40:48]
        lem = m[:, 48:80]
        emax = m[:, 80:88]
        msk = m[:, 88:120]
        sc_ = m[:, 120:160]
        kb.memset("dve", m[:, 4:8], -1e30, B)
        kb.tt("dve", m[:, 0:4], pl[:, 0:4], rb[:, 0:4], ALU.add, [b_pl, bWr], B)
        kb.tt("dve", le, pl[:, 4:36], rb[:, 4:36], ALU.add, [b_pl, bWr], B)
        kb.op("dve", lambda e, lg8=lg8, gmax=gmax: e.max(out=gmax, in_=lg8), B, B)
        kb.op("dve", lambda e, k=k, lg8=lg8, gmax=gmax: e.max_index(out=smu[k][:, 0:8], in_max=gmax, in_values=lg8), B, B)
        kb.ts("dve", sc_[:, 0:1], gmax[:, 0:1], -1.0, None, ALU.mult, None, B, B)
        kb.act(sc_[:, 8:12], m[:, 0:4], AF.Exp, B, B, bias=sc_[:, 0:1], accum_out=sc_[:, 1:2])
        kb.op("dve", lambda e, sc_=sc_: e.reciprocal(out=sc_[:, 2:3], in_=sc_[:, 1:2]), B, B)
        kb.copy("dve", sc_[:, 3:4], smu[k][:, 0:1], B, B)
        kb.ts("dve", msk, egrp[:], sc_[:, 3:4], None, ALU.is_equal, None, B + [bce], B)
        kb.ts("dve", msk, msk, 1e30, -1e30, ALU.mult, ALU.add, B, B)
        kb.tt("dve", lem, le, msk, ALU.add, B, B)
        kb.op("dve", lambda e, lem=lem, emax=emax: e.max(out=emax, in_=lem), B, B)
        kb.op("dve", lambda e, k=k, lem=lem, emax=emax: e.max_index(out=smu[k][:, 8:16], in_max=emax, in_values=lem), B, B)
        kb.tt("dve", sc_[:, 4:5], emax[:, 0:1], emax[:, 1:2], ALU.subtract, B, B)
        kb.act(sc_[:, 5:6], sc_[:, 4:5], AF.Sigmoid, B, B)
        kb.ts("dve", sc_[:, 6:7], sc_[:, 5:6], -1.0, 1.0, ALU.mult, ALU.add, B, B)
        kb.ts("dve", wts[:, i, :], sc_[:, 5:7], sc_[:, 2:3], None, ALU.mult, None, B, [b_wts])
        kb.copy("dve", sc_[:, 12:14], smu[k][:, 8:10], B, B)
        M0 = m[:, 88:120]
        M1 = m[:, 48:80]
        kb.ts("dve", M0, eio[:], sc_[:, 12:13], None, ALU.is_equal, None, B + [bce], B)
        kb.ts("dve", M1, eio[:], sc_[:, 13:14], None, ALU.is_equal, None, B + [bce], B)
        Mb = m[:, 8:40]
        kb.tt("dve", Mb, M0, M1, ALU.add, B, B)
        kb.mm(pp[:, 0:32], c["sup"][:], Mb, True, True, B + [kb.bc], [b_pp])
        kb.mm(pp[:, 32:64], c["onesf"][:], Mb, True, True, B + [kb.bc], [b_pp])
        rbase = m[:, 8:40]
        kb.tt("dve", rbase, pp[:, 0:32], cnt[:], ALU.add, [b_pp, b_cnt], B)
        kb.tt("dve", cnt[:], cnt[:], pp[:, 32:64], ALU.add, [b_pp, b_cnt], [b_cnt])
        kb.tt("dve", M0, M0, rbase, ALU.mult, B, B)
        kb.tt("dve", M1, M1, rbase, ALU.mult, B, B)
        kb.op("dve", lambda e, M0=M0, sc_=sc_: e.tensor_reduce(out=sc_[:, 14:15], in_=M0, axis=AX.X, op=ALU.add), B, B)
        kb.op("dve", lambda e, M1=M1, sc_=sc_: e.tensor_reduce(out=sc_[:, 15:16], in_=M1, axis=AX.X, op=ALU.add), B, B)
        kb.stt("dve", sc_[:, 16:18], sc_[:, 12:14], float(CAP), sc_[:, 14:16], ALU.mult, ALU.add, B, B)
        kb.ts("dve", sc_[:, 18:20], sc_[:, 14:16], float(CAP), None, ALU.is_ge, None, B, B)
        kb.stt("dve", sc_[:, 16:18], sc_[:, 18:20], float(OOB_IDX), sc_[:, 16:18], ALU.mult, ALU.add, B, B)
        kb.copy("dve", smi[k][:, 0:2], sc_[:, 16:18], B, B)
        for kk in range(2):
            kb.P.dma("pool", lambda e, k=k, kk=kk, i=i: e.indirect_dma_start(
                out=T["rowasg"], out_offset=bass.IndirectOffsetOnAxis(ap=smi[k][:, kk:kk + 1], axis=0),
                in_=aid_all[:, i, kk, :], in_offset=None, bounds_check=breg(e, NE * CAP - 1), oob_is_err=False), B + [bce], [T["b_rowasg"]])
        recs.append(kb.P.rec)
        kb.P.rec = None
        if len(recs) == 2:
            kb.P.replay_interleaved(recs, skew=6)
            recs = []
    cnti = kb.sb([1, 32], I32, "cnti")
    kb.copy("dve", cnti[:], cnt[0:1, :], [b_cnt], [b_cnt])
    kb.dma(T["cnt_d"], cnti[:], r=[b_cnt], w=[T["b_cntd"]])
    kb.pop()
    kb.push()
    cregs = {}

    def load_cnt(e):
        for en in Prog.ENGS:
            def ld(eo, e=e):
                key = (id(eo), e % 2)
                if key not in cregs:
                    cregs[key] = eo.alloc_register("moecnt%d_%d_%d" % (l, e % 2, len(cregs)))
                return eo.reg_load(cregs[key], T["cnt_d"][0:1, e:e + 1])
            kb.P.op(en, ld, [T["b_cntd"]], ())

    def cond_for(e, s_):
        def cond(eo):
            return eo.If_cmp(cregs[(id(eo), e % 2)], s_ * 128, "IS_GT")
        return cond
    Wg = [kb.sb([128, 8, DE], BF16, "Wg") for _ in range(2)]
    Wu = [kb.sb([128, 8, DE], BF16, "Wu") for _ in range(2)]
    Wd = [kb.sb([128, 4, D], BF16, "Wd") for _ in range(2)]
    b_Wg = [Buf(), Buf()]
    b_Wu = [Buf(), Buf()]
    b_Wd = [Buf(), Buf()]
    idx = [kb.sb([128, 2], I32, "idx") for _ in range(3)]
    b_idx = [Buf() for _ in range(3)]
    Xg = [kb.sb([128, D], BF16, "Xg") for _ in range(3)]
    b_Xg = [Buf() for _ in range(3)]
    for j in range(3):
        kb.memset("dve", Xg[j][:], 0.0, [b_Xg[j]])
    XgT = [kb.sb([128, 8, 128], BF16, "XgT") for _ in range(2)]
    b_XgT = [Buf(), Buf()]
    pT = kb.ps([128, 8, 128], BF16, "pT")
    b_pT = Buf()
    pg = kb.ps([128, 512], F32, "pg")
    b_pg = Buf()
    pu = kb.ps([128, 512], F32, "pu")
    b_pu = Buf()
    py = [kb.ps([128, 512], F32, "py") for _ in range(2)]
    b_py = [Buf(), Buf()]
    sgm = [kb.sb([128, 512], F32, "sgm") for _ in range(2)]
    av = [kb.sb([128, 512], BF16, "av") for _ in range(2)]
    b_av = [Buf(), Buf()]
    aT = [kb.sb([128, 4, 128], BF16, "aT") for _ in range(2)]
    b_aT = [Buf(), Buf()]
    yr = [kb.sb([128, D], F32, "yr") for _ in range(2)]
    b_yr = [Buf(), Buf()]

    stg = [kb.sb([128, 8, DE], F32, "stg_g"), kb.sb([128, 8, DE], F32, "stg_u"), kb.sb([128, 4, D], F32, "stg_d")]
    b_stg = [[Buf(), Buf(), Buf()], [Buf(), Buf(), Buf()]]

    def load_expert_dma(e):
        k = e % 2
        st = stg
        kb.dma(st[0][:], T["moe_w_gate"][l, e].rearrange("(c p) n -> p c n", p=128), w=[b_stg[0][0]])
        kb.dma(st[1][:], T["moe_w_up"][l, e].rearrange("(c p) n -> p c n", p=128), w=[b_stg[0][1]])
        kb.dma(st[2][:], T["moe_w_down"][l, e].rearrange("(c p) n -> p c n", p=128), w=[b_stg[0][2]])

    def cast_expert(e):
        k = e % 2
        st = stg
        kb.copy("act", Wg[k][:], st[0][:], [b_stg[0][0]], [b_Wg[k]])
        kb.copy("dve", Wu[k][:], st[1][:], [b_stg[0][1]], [b_Wu[k]])
        kb.copy("dve", Wd[k][:], st[2][:], [b_stg[0][2]], [b_Wd[k]])

    def gather(n):
        e, s_ = divmod(n, NSLOT)
        j = n % 3
        r0 = e * CAP + s_ * 128
        kb.dma(idx[j][:], T["rowasg"][r0:r0 + 128, :], r=[T["b_rowasg"]], w=[b_idx[j]])
        kb.P.dma("pool", lambda en, j=j: en.indirect_dma_start(
            out=Xg[j][:], out_offset=None, in_=T["hd"], in_offset=bass.IndirectOffsetOnAxis(ap=idx[j][:, 0:1], axis=0),
            bounds_check=breg(en, 2 * S - 1), oob_is_err=False), [b_idx[j], T["b_hd"]], [b_Xg[j]])

    load_expert_dma(0)
    cast_expert(0)
    load_cnt(0)
    kb.P.begin_guard(cond_for(0, 0))
    gather(0)
    kb.P.end_guard()
    ntot = NE * NSLOT
    for n in range(ntot):
        e, s_ = divmod(n, NSLOT)
        kw = e % 2
        j = n % 3
        k2 = n % 2
        if s_ == 0 and e + 1 < NE:
            load_cnt(e + 1)
        if s_ == 0 and e + 1 < NE:
            load_expert_dma(e + 1)
        if n + 1 < ntot:
            e1, s1 = divmod(n + 1, NSLOT)
            kb.P.begin_guard(cond_for(e1, s1))
            gather(n + 1)
            kb.P.end_guard()
        kb.P.begin_guard(cond_for(e, s_))
        for cc in range(8):
            kb.tr(pT[:, cc, :], Xg[j][:, cc * 128:(cc + 1) * 128], c["idb"][:], [b_Xg[j], kb.bc], [b_pT])
        kb.copy("act", XgT[k2][:], pT[:], [b_pT], [b_XgT[k2]])
        for cc in range(8):
            kb.mm(pg[:], XgT[k2][:, cc, :], Wg[kw][:, cc, :], cc == 0, cc == 7, [b_XgT[k2], b_Wg[kw]], [b_pg])
        for cc in range(8):
            kb.mm(pu[:], XgT[k2][:, cc, :], Wu[kw][:, cc, :], cc == 0, cc == 7, [b_XgT[k2], b_Wu[kw]], [b_pu])
        kb.act(sgm[k2][:], pg[:], AF.Sigmoid, [b_pg], [b_av[k2]])
        kb.tt("dve", sgm[k2][:], sgm[k2][:], pg[:], ALU.mult, [b_pg, b_av[k2]], [b_av[k2]])
        kb.tt("dve", av[k2][:], sgm[k2][:], pu[:], ALU.mult, [b_pu, b_av[k2]], [b_av[k2]])
        for cc in range(4):
            kb.tr(pT[:, cc, :], av[k2][:, cc * 128:(cc + 1) * 128], c["idb"][:], [b_av[k2], kb.bc], [b_pT])
        kb.copy("act", aT[k2][:], pT[:, 0:4, :], [b_pT], [b_aT[k2]])
        for nn in range(2):
            for cc in range(4):
                kb.mm(py[nn][:], aT[k2][:, cc, :], Wd[kw][:, cc, nn * 512:(nn + 1) * 512], cc == 0, cc == 3, [b_aT[k2], b_Wd[kw]], [b_py[nn]])
        kb.copy("act", yr[k2][:, 0:512], py[0][:], [b_py[0]], [b_yr[k2]])
        kb.copy("dve", yr[k2][:, 512:1024], py[1][:], [b_py[1]], [b_yr[k2]])
        kb.P.dma("pool", lambda en, j=j, k2=k2: en.indirect_dma_start(
            out=T["Yd"], out_offset=bass.IndirectOffsetOnAxis(ap=idx[j][:, 0:1], axis=0), in_=yr[k2][:], in_offset=None,
            bounds_check=breg(en, 2 * S - 1), oob_is_err=False), [b_idx[j], b_yr[k2]], [T["b_Yd"]])
        kb.P.end_guard()
        if s_ == NSLOT - 1 and e + 1 < NE:
            cast_expert(e + 1)
    kb.pop()
    kb.push()
    G = kb.sb([128, D], F32, "G2")
    bG = Buf()
    base = l * 6144 + 5120
    kb.dma(G[:], T["modd"][0:1, base:base + 1024].partition_broadcast(128), r=[T["b_modd"]], w=[bG])
    y0 = [kb.sb([128, D], F32, "y0") for _ in range(2)]
    y1 = [kb.sb([128, D], F32, "y1") for _ in range(2)]
    xt = [kb.sb([128, D], F32, "xt3") for _ in range(2)]
    b_in = [Buf(), Buf()]
    acc = [kb.sb([128, D], F32, "acc") for _ in range(2)]
    b_acc = [Buf(), Buf()]
    for i in range(NT):
        k = i % 2
        kb.dma(y0[k][:], T["Yd"][i * 128:(i + 1) * 128, :], r=[T["b_Yd"]], w=[b_in[k]])
        kb.dma(y1[k][:], T["Yd"][S + i * 128:S + (i + 1) * 128, :], r=[T["b_Yd"]], w=[b_in[k]])
        kb.dma(xt[k][:], xsrc[i * 128:(i + 1) * 128, :], r=[bx[i]], w=[b_in[k]])
        kb.ts("dve", acc[k][:], y0[k][:], wts[:, i, 0:1], None, ALU.mult, None, [b_in[k], b_wts], [b_acc[k]])
        kb.stt("dve", acc[k][:], y1[k][:], wts[:, i, 1:2], acc[k][:], ALU.mult, ALU.add, [b_in[k], b_wts, b_acc[k]], [b_acc[k]])
        kb.tt("dve", acc[k][:], acc[k][:], G[:], ALU.mult, [b_acc[k], bG], [b_acc[k]])
        kb.tt("dve", acc[k][:], acc[k][:], xt[k][:], ALU.add, [b_acc[k], b_in[k]], [b_acc[k]])
        kb.dma(xdst[i * 128:(i + 1) * 128, :], acc[k][:], r=[b_acc[k]], w=[bxd[i]])
    kb.pop()
    kb.pop()


def _headnorm_consts(kb, gsrc, extra_scale):
    blk = kb.sb([128, 128], BF16, "blk64")
    b = Buf()
    kb.memset("dve", blk[:], 0.0, [b])
    kb.memset("dve", blk[0:64, 0:64], 1.0, [b])
    kb.memset("dve", blk[64:128, 64:128], 1.0, [b])
    gcol = kb.sb([128, 1], F32, "gcol")
    kb.dma(gcol[0:64, :], gsrc.rearrange("o d -> d o"), w=[b])
    kb.dma(gcol[64:128, :], gsrc.rearrange("o d -> d o"), w=[b])
    if extra_scale != 1.0:
        kb.ts("dve", gcol[:], gcol[:], float(extra_scale), None, ALU.mult, None, [b], [b])
    return blk, gcol, b


class HeadNormT:
    def __init__(self, kb, blk, gcol, bconst):
        self.kb = kb
        self.blk, self.gcol, self.bconst = blk, gcol, bconst
        self.sq = [kb.sb([128, 512], BF16, "hsq") for _ in range(2)]
        self.rn = [kb.sb([128, 512], F32, "hrn") for _ in range(2)]
        self.b = [Buf(), Buf()]
        self.pn = kb.ps([128, 512], F32, "hpn")
        self.b_pn = Buf()
        self.n = 0

    def run(self, out, pin, b_pin, b_out):
        k = self.s1(pin, b_pin)
        self.s2(k)
        self.s3(k, out, pin, b_pin, b_out)

    def s1(self, pin, b_pin):
        k = self.n % 2
        self.n += 1
        self.kb.act(self.sq[k][:], pin, AF.Square, [b_pin], [self.b[k]])
        return k

    def s2(self, k):
        self.kb.mm(self.pn[:], self.blk[:], self.sq[k][:], True, True, [self.b[k], self.bconst], [self.b_pn])

    def s3(self, k, out, pin, b_pin, b_out):
        kb = self.kb
        c = kb.c
        kb.act(self.rn[k][:], self.pn[:], AF.Sqrt, [self.b_pn, kb.bc], [self.b[k]], scale=1.0 / 64, bias=c["epsc"][:])
        kb.op("dve", lambda e: e.reciprocal(out=self.rn[k][:], in_=self.rn[k][:]), [self.b[k]], [self.b[k]])
        kb.stt("dve", out, pin, self.gcol[:, 0:1], self.rn[k][:], ALU.mult, ALU.mult, [b_pin, self.b[k], self.bconst], [b_out])


def phase_kv(kb, T, xsrc, bx):
    c = kb.c
    kb.push()
    A, Bt, G_unused, bmod = load_mod(kb, T, 0, "kv")
    Wkv = kb.sb([128, 8, 2064], BF16, "Wkv")
    bW = Buf()
    load_w_bf16(kb, Wkv, T["kv_w"], D, 2064, bW)
    blk, gcol, bconst = _headnorm_consts(kb, T["k_norm_g"], 1.0)
    hn = HeadNormT(kb, blk, gcol, bconst)
    fb = kb.sb([128, 16], F32, "fb")
    kb.dma(fb[:], T["kv_forget_b"].partition_broadcast(128), w=[bconst])
    nt = NormT(kb, A, Bt, bmod)
    hTg = [kb.sb([128, 8, 512], BF16, "hTg") for _ in range(2)]
    b_hTg = [Buf(), Buf()]
    pk = [kb.ps([128, 512], F32, "pk") for _ in range(3)]
    b_pk = [Buf() for _ in range(3)]
    psm = kb.ps([128, 512], F32, "psm")
    b_psm = Buf()
    kst = [kb.sb([128, 512], BF16, "kst") for _ in range(2)]
    b_kst = [Buf(), Buf()]
    vst = [kb.sb([128, 16, 128], BF16, "vst") for _ in range(2)]
    b_vst = [Buf(), Buf()]
    for j in range(2):
        kb.memset("dve", vst[j][:], 1.0, [b_vst[j]])
    carry = kb.sb([128, 16], F32, "carry")
    b_carry = Buf()
    kb.memset("dve", carry[:], 0.0, [b_carry])
    lf = [kb.sb([128, 16], F32, "lf") for _ in range(2)]
    fcm = [kb.sb([128, 16], F32, "fcm") for _ in range(2)]
    r1 = [kb.sb([128, 16], F32, "r1") for _ in range(2)]
    sp3 = [kb.sb([128, 2, 3, 16], BF16, "sp3") for _ in range(2)]
    spT = [kb.sb([48, 2, 128], BF16, "spT") for _ in range(2)]
    b_f = [Buf(), Buf()]
    pst = kb.ps([48, 2, 128], BF16, "pst")
    b_pst = Buf()
    onesr = kb.sb([48, 512], BF16, "onesr")
    kb.memset("dve", onesr[:], 1.0, [bconst])
    for r in range(3):
        for g in range(S // 512):
            kb.dma(T["kaug"][:, 64 + r, g * 512:(g + 1) * 512], onesr[0:16, :], r=[bconst], w=[T["b_kaug"]])
            kb.dma(T["qaug"][:, 67 + r, g * 512:(g + 1) * 512], onesr[0:16, :], r=[bconst], w=[T["b_qaug"]])
    nv = 0
    NG = S // 512
    kcnt = [0]

    def kchunk_gen(g, ch):
        kg = g % 2
        idx = kcnt[0]
        kcnt[0] += 1
        k3 = idx % 3
        k2 = idx % 2

        def gen():
            for cc in range(8):
                kb.mm(pk[k3][:], Wkv[:, cc, ch * 128:(ch + 1) * 128], hTg[kg][:, cc, :], cc == 0, cc == 7, [bW, b_hTg[kg]], [b_pk[k3]])
            yield
            kk = hn.s1(pk[k3][:], b_pk[k3])
            yield
            hn.s2(kk)
            yield
            hn.s3(kk, kst[k2][:], pk[k3][:], b_pk[k3], b_kst[k2])
            kb.dma(T["kaug"][2 * ch, 0:64, g * 512:(g + 1) * 512], kst[k2][0:64, :], r=[b_kst[k2]], w=[T["b_kaug"]])
            kb.dma(T["kaug"][2 * ch + 1, 0:64, g * 512:(g + 1) * 512], kst[k2][64:128, :], r=[b_kst[k2]], w=[T["b_kaug"]])
        return gen

    for t in range(4):
        nt.run(xsrc[t * 128:(t + 1) * 128, :], bx[t], hTg[0][:, :, t * 128:(t + 1) * 128], b_hTg[0])
    for g in range(NG):
        kg = g % 2
        pend = {}

        def extra(r, g=g):
            if g + 1 >= NG:
                return
            t, ph = divmod(r, 2)
            if t < 4 and ph == 0:
                i = (g + 1) * 4 + t
                pend[t] = nt.run_a(xsrc[i * 128:(i + 1) * 128, :], bx[i])
            if t < 4 and ph == 1:
                kn = (g + 1) % 2
                nt.run_b(pend[t], hTg[kn][:, :, t * 128:(t + 1) * 128], b_hTg[kn])

        pipeline([kchunk_gen(g, ch) for ch in range(8)], extra)
        for t in range(4):
            i = g * 4 + t
            kv = nv % 2
            nv += 1
            for n in range(2):
                for cc in range(8):
                    kb.mm(pk[n][:], hTg[kg][:, cc, t * 128:(t + 1) * 128], Wkv[:, cc, 1024 + n * 512:1024 + (n + 1) * 512],
                          cc == 0, cc == 7, [b_hTg[kg], bW], [b_pk[n]])
            for cc in range(8):
                kb.mm(psm[:, 0:16], hTg[kg][:, cc, t * 128:(t + 1) * 128], Wkv[:, cc, 2048:2064], cc == 0, cc == 7, [b_hTg[kg], bW], [b_psm])
            for n in range(2):
                eng = "act" if n == 0 else "dve"
                kb.copy(eng, vst[kv][:, n * 8:(n + 1) * 8, 0:64], pk[n][:].rearrange("p (h d) -> p h d", h=8), [b_pk[n]], [b_vst[kv]])
            kb.dma(T["vaug"][:, i * 128:(i + 1) * 128, :].rearrange("h t d -> t h d"), vst[kv][:], r=[b_vst[kv]], w=[T["b_vaug"]])
            B = [b_f[kv]]
            kb.tt("dve", lf[kv][:], psm[:, 0:16], fb[:], ALU.add, [b_psm, bconst], B)
            kb.act(lf[kv][:], lf[kv][:], AF.Exp, B, B, scale=-1.0)
            kb.act(lf[kv][:], lf[kv][:], AF.Ln, B + [kb.bc], B, bias=c["onec"][:])
            kb.ts("dve", lf[kv][:], lf[kv][:], -1.0, None, ALU.mult, None, B, B)
            kb.mm(psm[:, 16:32], c["utri"][:], lf[kv][:], True, True, B + [kb.bc], [b_psm])
            kb.mm(psm[:, 32:48], c["onesf"][:], lf[kv][:], True, True, B + [kb.bc], [b_psm])
            kb.tt("dve", fcm[kv][:], psm[:, 16:32], carry[:], ALU.add, [b_psm, b_carry], B)
            kb.tt("dve", carry[:], carry[:], psm[:, 32:48], ALU.add, [b_psm, b_carry], [b_carry])
            kb.dma(T["fcum_d"][i * 128:(i + 1) * 128, :], fcm[kv][:], r=B, w=[T["b_fcum"]])
            kb.copy("dve", sp3[kv][:, 0, 0, :], fcm[kv][:], B, B)
            kb.tt("dve", r1[kv][:], fcm[kv][:], sp3[kv][:, 0, 0, :], ALU.subtract, B, B)
            kb.copy("dve", sp3[kv][:, 0, 1, :], r1[kv][:], B, B)
            kb.tt("dve", r1[kv][:], r1[kv][:], sp3[kv][:, 0, 1, :], ALU.subtract, B, B)
            kb.copy("dve", sp3[kv][:, 0, 2, :], r1[kv][:], B, B)
            kb.ts("dve", sp3[kv][:, 1, :, :], sp3[kv][:, 0, :, :], -1.0, None, ALU.mult, None, B, B)
            for qk in range(2):
                kb.tr(pst[:, qk, :], sp3[kv][:, qk, :, :].rearrange("p r h -> p (r h)"), c["idb"][:], B + [kb.bc], [b_pst])
            kb.copy("act", spT[kv][:], pst[:], [b_pst], B)
            for r in range(3):
                kb.dma(T["qaug"][:, 64 + r, i * 128:(i + 1) * 128], spT[kv][r * 16:(r + 1) * 16, 0, :], r=B, w=[T["b_qaug"]])
                kb.dma(T["kaug"][:, 67 + r, i * 128:(i + 1) * 128], spT[kv][r * 16:(r + 1) * 16, 1, :], r=B, w=[T["b_kaug"]])
    kb.pop()


def phase_fox_proj(kb, T, l, xsrc, bx):
    c = kb.c
    j = l - 2
    kb.push()
    A, Bt, G_unused, bmod = load_mod(kb, T, l, "mix")
    Wqz = kb.sb([128, 8, 2048], BF16, "Wqz")
    bW = Buf()
    load_w_bf16(kb, Wqz, T["fox_w_qz"][j], D, 2048, bW)
    blk, gcol, bconst = _headnorm_consts(kb, T["fox_q_norm_g"][j:j + 1, :], 64.0 ** -0.5)
    hn = HeadNormT(kb, blk, gcol, bconst)
    nt = NormT(kb, A, Bt, bmod)
    hTg = [kb.sb([128, 8, 512], BF16, "hTg") for _ in range(2)]
    b_hTg = [Buf(), Buf()]
    pq = [kb.ps([128, 512], F32, "pq") for _ in range(3)]
    b_pq = [Buf() for _ in range(3)]
    qst = [kb.sb([128, 512], BF16, "qst") for _ in range(4)]
    b_qst = [Buf() for _ in range(4)]
    NG = S // 512
    cnt = [0]

    def chunk_gen(g, ch):
        kg = g % 2
        idx = cnt[0]
        cnt[0] += 1
        k3 = idx % 3
        k4 = idx % 4

        def gen():
            for cc in range(8):
                kb.mm(pq[k3][:], Wqz[:, cc, ch * 128:(ch + 1) * 128], hTg[kg][:, cc, :], cc == 0, cc == 7, [bW, b_hTg[kg]], [b_pq[k3]])
            yield
            if ch >= 8:
                kb.act(qst[k4][:], pq[k3][:], AF.Sigmoid, [b_pq[k3]], [b_qst[k4]])
                kb.dma(T["szT"][ch - 8, :, g * 512:(g + 1) * 512], qst[k4][:], r=[b_qst[k4]], w=[T["b_szT"]])
                return
            kk = hn.s1(pq[k3][:], b_pq[k3])
            yield
            hn.s2(kk)
            yield
            hn.s3(kk, qst[k4][:], pq[k3][:], b_pq[k3], b_qst[k4])
            kb.dma(T["qaug"][2 * ch, 0:64, g * 512:(g + 1) * 512], qst[k4][0:64, :], r=[b_qst[k4]], w=[T["b_qaug"]])
            kb.dma(T["qaug"][2 * ch + 1, 0:64, g * 512:(g + 1) * 512], qst[k4][64:128, :], r=[b_qst[k4]], w=[T["b_qaug"]])
        return gen

    for t in range(4):
        nt.run(xsrc[t * 128:(t + 1) * 128, :], bx[t], hTg[0][:, :, t * 128:(t + 1) * 128], b_hTg[0])
    for g in range(NG):
        pend = {}

        def extra(r, g=g):
            if g + 1 >= NG:
                return
            t, ph = divmod(r, 4)
            if t < 4 and ph == 1:
                i = (g + 1) * 4 + t
                pend[t] = nt.run_a(xsrc[i * 128:(i + 1) * 128, :], bx[i])
            if t < 4 and ph == 3:
                kn = (g + 1) % 2
                nt.run_b(pend[t], hTg[kn][:, :, t * 128:(t + 1) * 128], b_hTg[kn])

        pipeline([chunk_gen(g, ch) for ch in range(16)], extra)
    kb.pop()


def phase_fox_attn(kb, T, l):
    c = kb.c
    kb.push()
    ka = [kb.sb([70, S], BF16, "ka") for _ in range(2)]
    qa = [kb.sb([70, S], BF16, "qa") for _ in range(2)]
    va = [kb.sb([128, NT, 128], BF16, "va") for _ in range(2)]
    sz = [kb.sb([64, S], BF16, "sz") for _ in range(2)]
    b_in = [Buf(), Buf()]
    ps_ = [kb.ps([128, 512], F32, "ps") for _ in range(3)]
    b_ps = [Buf() for _ in range(3)]
    po = [kb.ps([128, 512], F32, "po") for _ in range(2)]
    b_po = [Buf(), Buf()]
    pT = [kb.sb([128, 512], BF16, "pT") for _ in range(3)]
    b_pT = [Buf() for _ in range(3)]
    rl = [kb.sb([128, 512], F32, "rl") for _ in range(2)]
    on = [kb.sb([64, 512], F32, "on") for _ in range(2)]
    og = [kb.sb([64, 512], BF16, "og") for _ in range(2)]
    b_o = [Buf(), Buf()]

    def loads(h):
        k = h % 2
        kb.dma(ka[k][:], T["kaug"][h], r=[T["b_kaug"]], w=[b_in[k]])
        kb.dma(qa[k][:], T["qaug"][h], r=[T["b_qaug"]], w=[b_in[k]])
        kb.dma(va[k][:], T["vaug"][h].rearrange("(t p) d -> p t d", p=128), r=[T["b_vaug"]], w=[b_in[k]])
        ch, half = divmod(h, 2)
        kb.dma(sz[k][:], T["szT"][ch, half * 64:(half + 1) * 64, :], r=[T["b_szT"]], w=[b_in[k]])

    loads(0)
    LA = 2
    ng = 0
    n = 0
    for h in range(FH):
        k = h % 2
        if h + 1 < FH:
            loads(h + 1)
        pairs = []
        for g in range(S // 512):
            nk = 4 * g + 4
            for kt in range(nk):
                pairs.append((g, kt, nk))
        kgs = {}
        for g in range(S // 512):
            kgs[g] = ng % 2
            ng += 1
        slot = {}

        def emit_s(pi):
            g, kt, nk = pairs[pi]
            k3 = (n + pi) % 3
            d = kt - 4 * g
            q0 = g * 512 + (d * 128 if d > 0 else 0)
            w = (g + 1) * 512 - q0
            diag = d >= 0
            kb.mm(ps_[k3][:, 0:w], ka[k][:, kt * 128:(kt + 1) * 128], qa[k][:, q0:q0 + w], True, not diag, [b_in[k]], [b_ps[k3]])
            if diag:
                kb.mm(ps_[k3][:, 0:128], c["idb"][:], c["negmb"][:], False, True, [kb.bc], [b_ps[k3]])
            kb.act(pT[k3][:, 0:w], ps_[k3][:, 0:w], AF.Exp, [b_ps[k3]], [b_pT[k3]])

        def emit_o(pi):
            g, kt, nk = pairs[pi]
            k3 = (n + pi) % 3
            kg = kgs[g]
            d = kt - 4 * g
            q0 = g * 512 + (d * 128 if d > 0 else 0)
            w = (g + 1) * 512 - q0
            c0 = q0 - g * 512
            kb.mm(po[kg][:, c0:c0 + w], va[k][:, kt, :], pT[k3][:, 0:w], kt == 0, kt == nk - 1, [b_in[k], b_pT[k3]], [b_po[kg]])
            if kt == nk - 1:
                kb.op("dve", lambda e, kg=kg: e.reciprocal(out=rl[kg][64:128, :], in_=po[kg][64:128, :]), [b_po[kg]], [b_o[kg]])
                kb.tt("dve", on[kg][:], po[kg][0:64, :], rl[kg][64:128, :], ALU.mult, [b_po[kg], b_o[kg]], [b_o[kg]])
                kb.tt("dve", og[kg][:], on[kg][:], sz[k][:, g * 512:(g + 1) * 512], ALU.mult, [b_o[kg], b_in[k]], [b_o[kg]])
                kb.dma(T["ogT"][h * 64:(h + 1) * 64, g * 512:(g + 1) * 512], og[kg][:], r=[b_o[kg]], w=[T["b_ogT"]])

        npairs = len(pairs)
        for pi in range(npairs + LA):
            if pi < npairs:
                emit_s(pi)
            if pi - LA >= 0:
                emit_o(pi - LA)
        n += npairs
    kb.pop()


def phase_fox_out(kb, T, l, xsrc, bx, xdst, bxd):
    c = kb.c
    j = l - 2
    kb.push()
    G = kb.sb([128, D], F32, "G")
    bG = Buf()
    base = l * 6144 + 2048
    kb.dma(G[:], T["modd"][0:1, base:base + 1024].partition_broadcast(128), r=[T["b_modd"]], w=[bG])
    Wo = kb.sb([128, 8, D], BF16, "Wo")
    bW = Buf()
    load_w_bf16(kb, Wo, T["fox_w_out"][j], D, D, bW)
    ot = [kb.sb([128, 8, 128], BF16, "ot") for _ in range(2)]
    xt = [kb.sb([128, D], F32, "xt") for _ in range(2)]
    b_in = [Buf(), Buf()]
    py = [kb.ps([128, 512], F32, "py") for _ in range(2)]
    b_py = [Buf(), Buf()]
    yt = [kb.sb([128, D], F32, "yt") for _ in range(2)]
    b_yt = [Buf(), Buf()]
    for i in range(NT):
        k = i % 2
        kb.dma(ot[k][:], T["ogT"][:, i * 128:(i + 1) * 128].rearrange("(c p) s -> p c s", p=128), r=[T["b_ogT"]], w=[b_in[k]])
        kb.dma(xt[k][:], xsrc[i * 128:(i + 1) * 128, :], r=[bx[i]], w=[b_in[k]])
        for n in range(2):
            for cc in range(8):
                kb.mm(py[n][:], ot[k][:, cc, :], Wo[:, cc, n * 512:(n + 1) * 512], cc == 0, cc == 7, [b_in[k], bW], [b_py[n]])
            kb.tt("dve", yt[k][:, n * 512:(n + 1) * 512], py[n][:], G[:, n * 512:(n + 1) * 512], ALU.mult, [b_py[n], bG], [b_yt[k]])
        kb.tt("dve", yt[k][:], yt[k][:], xt[k][:], ALU.add, [b_yt[k], b_in[k]], [b_yt[k]])
        kb.dma(xdst[i * 128:(i + 1) * 128, :], yt[k][:], r=[b_yt[k]], w=[bxd[i]])
    kb.pop()


IN_SHAPES = {
    "x": [S, D], "c": [1, D], "mod_w": [4, D, 6144], "mod_b": [4, 6144], "norm_mix_g": [4, D], "norm_ffn_g": [4, D],
    "gdn_w_in": [2, D, GPROJ], "gdn_conv_w": [2, 4, 3072], "gdn_a_log": [2, 8], "gdn_dt_bias": [2, 8],
    "gdn_out_norm_g": [2, 128], "gdn_w_out": [2, D, D], "kv_mod_w": [D, 2048], "kv_mod_b": [1, 2048],
    "kv_norm_g": [1, D], "kv_w": [D, 2064], "kv_forget_b": [1, 16], "k_norm_g": [1, 64],
    "fox_w_qz": [2, D, 2048], "fox_q_norm_g": [2, 64], "fox_w_out": [2, D, D],
    "moe_w_group": [4, D, 4], "moe_b_group": [4, 4], "moe_w_expert": [4, D, 32], "moe_b_expert": [4, 32],
    "moe_w_gate": [4, 32, D, DE], "moe_w_up": [4, 32, D, DE], "moe_w_down": [4, 32, DE, D],
}


class LazyT(dict):
    def __init__(self, kb, lazy):
        super().__init__()
        self.kb = kb
        self.lazy = lazy
        self.declared = []

    def __missing__(self, k):
        if k in IN_SHAPES:
            v = self.kb.din(k, IN_SHAPES[k])
            self[k] = v
            self.declared.append(k)
            return v
        raise KeyError(k)


def build(stop=None, dbg=(), lazy=False, stop2=None):
    kb = KB(dbg)
    T = LazyT(kb, lazy)
    kb.T = T
    if not lazy:
        for k in IN_SHAPES:
            T[k]
    T["y"] = kb.dout("y", [S, D])
    T["b_y"] = [Buf() for _ in range(NT)]
    T["modd"] = kb.dscr("modd", [1, 6144 * 4 + 2048])
    T["b_modd"] = Buf()
    T["xs"] = kb.dscr("xs", [S, D])
    T["b_xs"] = [Buf() for _ in range(NT)]
    T["b_xin"] = [Buf() for _ in range(NT)]
    T["qkv_d"] = kb.dscr("qkv_d", [NT, 128, 24 * 128], BF16)
    T["b_qkv"] = [Buf() for _ in range(NT)]
    T["sz_d"] = kb.dscr("sz_d", [S, D], BF16)
    T["b_sz"] = [Buf() for _ in range(NT)]
    T["rowasg"] = kb.dscr("rowasg", [NE * CAP, 2], I32)
    T["b_rowasg"] = Buf()
    T["cnt_d"] = kb.dscr("cnt_d", [1, 32], I32)
    T["b_cntd"] = Buf()
    T["hd"] = kb.dscr("hd", [2 * S, D], BF16)
    T["b_hd"] = Buf()
    T["Yd"] = kb.dscr("Yd", [2 * S, D], F32)
    T["b_Yd"] = Buf()
    T["kaug"] = kb.dscr("kaug", [FH, 70, S], BF16)
    T["b_kaug"] = Buf()
    T["qaug"] = kb.dscr("qaug", [FH, 70, S], BF16)
    T["b_qaug"] = Buf()
    T["vaug"] = kb.dscr("vaug", [FH, S, 128], BF16)
    T["b_vaug"] = Buf()
    T["fcum_d"] = kb.dscr("fcum_d", [S, 16], F32)
    T["b_fcum"] = Buf()
    T["szT"] = kb.dscr("szT", [8, 128, S], BF16)
    T["b_szT"] = Buf()
    T["ogT"] = kb.dscr("ogT", [D, S], BF16)
    T["b_ogT"] = Buf()
    T["gb_d"] = kb.dscr("gb_d", [S, 16], F32)
    T["b_gb"] = [Buf() for _ in range(NT)]
    kb.make_consts()
    if stop == "kvtest":
        phase_mod(kb, T)
        phase_kv(kb, T, T["x"], T["b_xin"])
        if dbg and "kaug" in dbg and stop2 == "kv":
            return finish(kb, T)
        phase_fox_proj(kb, T, 2, T["x"], T["b_xin"])
        phase_fox_attn(kb, T, 2)
        phase_fox_out(kb, T, 2, T["x"], T["b_xin"], T["xs"], T["b_xs"])
        return finish(kb, T, dump_x=True)
    phase_mod(kb, T)
    if stop == "mod":
        return finish(kb, T)
    xsrc, bx = T["x"], T["b_xin"]
    for l in range(DEPTH):
        last = l == DEPTH - 1
        if l < 2:
            phase_gdn_proj(kb, T, l, xsrc, bx)
            if stop == "gdnproj":
                return finish(kb, T)
            phase_gdn_delta(kb, T, l, xsrc, bx, T["xs"], T["b_xs"])
        else:
            phase_fox_proj(kb, T, l, xsrc, bx)
            phase_fox_attn(kb, T, l)
            phase_fox_out(kb, T, l, xsrc, bx, T["xs"], T["b_xs"])
        xsrc, bx = T["xs"], T["b_xs"]
        if stop == "mix%d" % l:
            return finish(kb, T, dump_x=True)
        if last:
            phase_moe(kb, T, l, xsrc, bx, T["y"], T["b_y"])
        else:
            phase_moe(kb, T, l, xsrc, bx, T["xs"], T["b_xs"])
        if stop == "moe%d" % l:
            return finish(kb, T, dump_x=True)
        if l == 1:
            phase_kv(kb, T, xsrc, bx)
    return finish(kb, T)


def finish(kb, T, dump_x=False):
    global LASTP, LASTT
    P = kb.P
    LASTP = P
    LASTT = T
    if dump_x:
        for i in range(NT):
            kb.dma(T["y"][i * 128:(i + 1) * 128, :], T["xs"][i * 128:(i + 1) * 128, :], r=[T["b_xs"][i]], w=[Buf()])
    P.barrier()
    P.check()
    P.emit()
    P.close()
    while kb.scopes:
        for cm in reversed(kb.scopes.pop()):
            cm.__exit__(None, None, None)
    return kb.nc


_NC_CACHE = {}


def kernel(**inputs):
    if "nc" not in _NC_CACHE:
        _REGS.clear()
        _NC_CACHE["nc"] = build()
    nc = _NC_CACHE["nc"]
    ncores = 8
    shared = {}
    for k, shp in IN_SHAPES.items():
        if k in ("x", "c"):
            continue
        shared[k] = np.ascontiguousarray(np.asarray(inputs[k], dtype=np.float32).reshape(shp))
    x = np.asarray(inputs["x"], dtype=np.float32)
    cc = np.asarray(inputs["c"], dtype=np.float32)
    in_maps = []
    for b in range(ncores):
        m = dict(shared)
        m["x"] = np.ascontiguousarray(x[b])
        m["c"] = np.ascontiguousarray(cc[b:b + 1])
        in_maps.append(m)
    res = run_bass_kernel_spmd(nc, in_maps, core_ids=list(range(ncores)))
    out = np.stack([np.asarray(r["y"], dtype=np.float32) for r in res.results], axis=0)
    return out
```

```python
import os
import numpy as np
import concourse.bass as bass
import concourse.mybir as mybir
from concourse.bass_utils import run_bass_kernel_spmd

F32 = mybir.dt.float32
BF16 = mybir.dt.bfloat16
I32 = mybir.dt.int32
U32 = mybir.dt.uint32
AF = mybir.ActivationFunctionType
ALU = mybir.AluOpType
AX = mybir.AxisListType

S = 4096
D = 1024
NT = S // 128
DEPTH = 4
EPS = 1e-6
GH = 8
GPROJ = 4112
FH = 16
NE = 32
DE = 512
NSLOT = 8
CAP = NSLOT * 128
NEG = -30000.0


class Buf:
    __slots__ = ("name", "last_w", "readers")

    def __init__(self, name=""):
        self.name = name
        self.last_w = None
        self.readers = {}


class Prog:
    ENGS = ("pe", "act", "dve", "pool", "sp")

    def __init__(self, nc, n_dma_sems=14):
        self.nc = nc
        self.streams = {e: [] for e in self.ENGS}
        self.sems = {}
        self.tick = {e: 0 for e in self.ENGS}
        self.waited = {e: {} for e in self.ENGS}
        self._ctx = []
        self.rec = None
        for e in self.ENGS:
            self._newsem("eng_" + e)
        self.dma_slots = {}
        self.n_dma_sems = n_dma_sems
        for q in ("sp", "act", "pool"):
            self.dma_slots[q] = {"next": 0, "count": [0] * n_dma_sems}
            for i in range(n_dma_sems):
                self._newsem("dma_%s_%d" % (q, i))

    def _newsem(self, key):
        cm = self.nc.semaphore(key)
        h = cm.__enter__()
        self._ctx.append(cm)
        self.sems[key] = h

    def close(self):
        for cm in reversed(self._ctx):
            cm.__exit__(None, None, None)

    def _collect(self, eng, reads, writes, is_dma=False):
        deps = {}

        def add(d):
            if deps.get(d[0], -1) < d[1]:
                deps[d[0]] = d[1]
        for b in reads:
            lw = b.last_w
            if lw is not None and (is_dma or not (lw[2] == eng and eng == "pe")):
                add(lw)
        for b in writes:
            if b.last_w is not None and (is_dma or b.last_w[2] != eng):
                add(b.last_w)
            for re_, d in b.readers.items():
                if is_dma or re_ != eng:
                    add(d)
        waits = []
        wd = self.waited[eng]
        for k, v in deps.items():
            if wd.get(k, -1) < v:
                wd[k] = v
                waits.append((k, v))
        return waits

    def _commit(self, eng, reads, writes, dep):
        for b in reads:
            b.readers[eng] = dep
        for b in writes:
            b.last_w = (dep[0], dep[1], eng)
            b.readers = {}

    def op(self, eng, fn, reads=(), writes=()):
        if self.rec is not None:
            self.rec.append(("op", eng, fn, tuple(reads), tuple(writes)))
            return
        waits = self._collect(eng, reads, writes)
        self.tick[eng] += 1
        key = "eng_" + eng
        self.streams[eng].append((waits, fn, key, 1))
        self._commit(eng, reads, writes, (key, self.tick[eng]))

    def dma(self, q, fn, reads=(), writes=()):
        if self.rec is not None:
            self.rec.append(("dma", q, fn, tuple(reads), tuple(writes)))
            return
        waits = self._collect(q, reads, writes, True)
        st = self.dma_slots[q]
        s = st["next"]
        st["next"] = (s + 1) % self.n_dma_sems
        key = "dma_%s_%d" % (q, s)
        prev = st["count"][s] * 16
        if prev > 0 and self.waited[q].get(key, -1) < prev:
            self.waited[q][key] = prev
            waits.append((key, prev))
        st["count"][s] += 1
        self.streams[q].append((waits, fn, key, 16))
        self._commit("dma_" + q + str(s), reads, writes, (key, st["count"][s] * 16))

    def replay_interleaved(self, lists, skew=4):
        units = []
        for lst in lists:
            u = []
            for item in lst:
                if item[0] == "op" and item[1] == "pe" and u and u[-1][-1][0] == "op" and u[-1][-1][1] == "pe":
                    u[-1].append(item)
                else:
                    u.append([item])
            units.append(u)
        pos = [0] * len(units)
        step = 0
        while any(pos[j] < len(units[j]) for j in range(len(units))):
            for j in range(len(units)):
                if step >= j * skew and pos[j] < len(units[j]):
                    for kind, a, fn, r, w in units[j][pos[j]]:
                        (self.op if kind == "op" else self.dma)(a, fn, r, w)
                    pos[j] += 1
            step += 1

    def begin_guard(self, cond):
        self._g_start = {e: len(self.streams[e]) for e in self.ENGS}
        self._g_waited = {e: dict(self.waited[e]) for e in self.ENGS}
        self._g_tick = dict(self.tick)
        self._g_dma = {q: list(st["count"]) for q, st in self.dma_slots.items()}
        self._g_cond = cond

    def end_guard(self):
        for e in self.ENGS:
            body = self.streams[e][self._g_start[e]:]
            del self.streams[e][self._g_start[e]:]
            if not body:
                continue
            incs = {}
            for waits, fn, key, inc in body:
                if fn is not None:
                    incs[key] = incs.get(key, 0) + inc
            base = {}
            for key in incs:
                if key.startswith("eng_"):
                    base[key] = self._g_tick[key[4:]]
                else:
                    _, q, sl = key.split("_")
                    base[key] = self._g_dma[q][int(sl)] * 16
            self.streams[e].append(("guard", self._g_cond, body, [(k, base[k], incs[k]) for k in incs]))
            self.waited[e] = self._g_waited[e]

    def barrier(self):
        targets = []
        for e in self.ENGS:
            if self.tick[e] > 0:
                targets.append(("eng_" + e, self.tick[e]))
        for q, st in self.dma_slots.items():
            for s, cnt in enumerate(st["count"]):
                if cnt > 0:
                    targets.append(("dma_%s_%d" % (q, s), cnt * 16))
        for e in self.ENGS:
            waits = []
            for k, v in targets:
                if k == "eng_" + e:
                    continue
                if self.waited[e].get(k, -1) < v:
                    self.waited[e][k] = v
                    waits.append((k, v))
            if waits:
                self.streams[e].append((waits, None, None, 0))

    def check(self):
        flat = {}
        for e in self.ENGS:
            out = []

            def walk(lst):
                for item in lst:
                    if item[0] == "guard":
                        walk(item[2])
                    else:
                        out.append(item)
            walk(self.streams[e])
            flat[e] = out
        sem = {k: 0 for k in self.sems}
        pos = {e: 0 for e in self.ENGS}
        progress = True
        while progress:
            progress = False
            for e in self.ENGS:
                lst = flat[e]
                while pos[e] < len(lst):
                    waits, fn, key, inc = lst[pos[e]]
                    if any(sem[k] < v for k, v in waits):
                        break
                    if fn is not None:
                        sem[key] += inc
                    pos[e] += 1
                    progress = True
        stuck = {e: (pos[e], len(flat[e])) for e in self.ENGS if pos[e] < len(flat[e])}
        if stuck:
            for e in stuck:
                waits = flat[e][pos[e]][0]
                print("STUCK", e, pos[e], [(k, v, sem[k]) for k, v in waits if sem[k] < v])
            raise RuntimeError("semaphore deadlock in generated program: %s" % stuck)

    def emit(self):
        nc = self.nc
        sems = self.sems
        streams = self.streams

        def run(engobj, lst):
            for item in lst:
                if item[0] == "guard":
                    _, cond, body, incs = item
                    with cond(engobj):
                        run(engobj, body)
                    with engobj.Else():
                        for k, basev, tot in incs:
                            if basev > 0:
                                engobj.wait_ge(sems[k], basev)
                            engobj.sem_inc(sems[k], tot)
                    continue
                waits, fn, key, inc = item
                for k, v in waits:
                    engobj.wait_ge(sems[k], v)
                if fn is not None:
                    fn(engobj).then_inc(sems[key], inc)

        with nc.Block() as block:
            @block.tensor
            def _(e):
                run(e, streams["pe"])

            @block.scalar
            def _(e):
                run(e, streams["act"])

            @block.vector
            def _(e):
                run(e, streams["dve"])

            @block.gpsimd
            def _(e):
                run(e, streams["pool"])

            @block.sync
            def _(e):
                run(e, streams["sp"])


class KB:
    def __init__(self, dbg=()):
        self.dbg = set(dbg)
        self.nc = bass.Bass("TRN2", target_bir_lowering=False)
        self.P = Prog(self.nc)
        self.scopes = [[]]
        self.uid = 0
        self.consts_ready = False

    def _nm(self, base):
        self.uid += 1
        return "%s_%d" % (base, self.uid)

    def sb(self, shape, dt, name="t"):
        cm = self.nc.sbuf_tensor(self._nm(name), list(shape), dt)
        t = cm.__enter__()
        self.scopes[-1].append(cm)
        return t

    def ps(self, shape, dt=F32, name="p"):
        cm = self.nc.psum_tensor(self._nm(name), list(shape), dt)
        t = cm.__enter__()
        self.scopes[-1].append(cm)
        return t

    def push(self):
        self.scopes.append([])

    def pop(self):
        self.P.barrier()
        for cm in reversed(self.scopes.pop()):
            cm.__exit__(None, None, None)

    def din(self, name, shape, dt=F32):
        return self.nc.dram_tensor(name, list(shape), dt, kind="ExternalInput").ap()

    def dout(self, name, shape, dt=F32):
        return self.nc.dram_tensor(name, list(shape), dt, kind="ExternalOutput").ap()

    def dscr(self, name, shape, dt=F32):
        if name in self.dbg:
            return self.nc.dram_tensor(name, list(shape), dt, kind="ExternalOutput").ap()
        return self.nc.dram_tensor(name, list(shape), dt, kind="Internal").ap()

    def op(self, eng, fn, r=(), w=()):
        self.P.op(eng, fn, r, w)

    def dma(self, out, in_, r=(), w=(), q="sp", **kw):
        self.P.dma(q, lambda e: e.dma_start(out=out, in_=in_, **kw), r, w)

    def mm(self, out, lhsT, rhs, start, stop, r=(), w=()):
        self.P.op("pe", lambda e: e.matmul(out, lhsT=lhsT, rhs=rhs, start=start, stop=stop), r, w)

    def tr(self, out, in_, ident, r=(), w=()):
        self.P.op("pe", lambda e: e.transpose(out=out, in_=in_, identity=ident), r, w)

    def act(self, out, in_, func, r=(), w=(), **kw):
        self.P.op("act", lambda e: e.activation(out=out, in_=in_, func=func, **kw), r, w)

    def tt(self, eng, out, in0, in1, op, r=(), w=()):
        self.P.op(eng, lambda e: e.tensor_tensor(out=out, in0=in0, in1=in1, op=op), r, w)

    def ts(self, eng, out, in0, s1, s2, op0, op1=None, r=(), w=()):
        if op1 is None:
            self.P.op(eng, lambda e: e.tensor_scalar(out=out, in0=in0, scalar1=s1, scalar2=None, op0=op0), r, w)
        else:
            self.P.op(eng, lambda e: e.tensor_scalar(out=out, in0=in0, scalar1=s1, scalar2=s2, op0=op0, op1=op1), r, w)

    def stt(self, eng, out, in0, scalar, in1, op0, op1, r=(), w=()):
        self.P.op(eng, lambda e: e.scalar_tensor_tensor(out=out, in0=in0, scalar=scalar, in1=in1, op0=op0, op1=op1), r, w)

    def copy(self, eng, out, in_, r=(), w=()):
        if eng == "act":
            self.P.op("act", lambda e: e.copy(out=out, in_=in_), r, w)
        else:
            self.P.op(eng, lambda e: e.tensor_copy(out=out, in_=in_), r, w)

    def memset(self, eng, ap, val, w=()):
        self.P.op(eng, lambda e: e.memset(ap, val), (), w)

    def make_consts(self):
        c = {}
        bc = Buf("consts")
        self.bc = bc
        idf = self.sb([128, 128], F32, "identf")
        self.memset("pool", idf[:], 0.0, [bc])
        self.op("pool", lambda e: e.affine_select(out=idf[:], in_=idf[:], pattern=[[-1, 128]], compare_op=ALU.not_equal,
                                                    fill=1.0, base=0, channel_multiplier=1), [bc], [bc])
        idb = self.sb([128, 128], BF16, "identb")
        self.copy("pool", idb[:], idf[:], [bc], [bc])
        onesf = self.sb([128, 128], F32, "onesf")
        self.memset("pool", onesf[:], 1.0, [bc])
        onesb = self.sb([128, 128], BF16, "onesb")
        self.memset("pool", onesb[:], 1.0, [bc])
        utri = self.sb([128, 128], F32, "utri")
        self.memset("pool", utri[:], 1.0, [bc])
        self.op("pool", lambda e: e.affine_select(out=utri[:], in_=utri[:], pattern=[[1, 128]], compare_op=ALU.is_ge,
                                                    fill=0.0, base=0, channel_multiplier=-1), [bc], [bc])
        sup = self.sb([128, 128], F32, "sup")
        self.memset("pool", sup[:], 1.0, [bc])
        self.op("pool", lambda e: e.affine_select(out=sup[:], in_=sup[:], pattern=[[1, 128]], compare_op=ALU.is_gt,
                                                    fill=0.0, base=0, channel_multiplier=-1), [bc], [bc])
        negm = self.sb([128, 128], F32, "negm")
        self.memset("pool", negm[:], 0.0, [bc])
        self.op("pool", lambda e: e.affine_select(out=negm[:], in_=negm[:], pattern=[[1, 128]], compare_op=ALU.is_ge,
                                                    fill=NEG, base=0, channel_multiplier=-1), [bc], [bc])
        negmb = self.sb([128, 128], BF16, "negmb")
        self.copy("pool", negmb[:], negm[:], [bc], [bc])
        epsc = self.sb([128, 1], F32, "epsc")
        self.memset("pool", epsc[:], EPS, [bc])
        onec = self.sb([128, 1], F32, "onec")
        self.memset("pool", onec[:], 1.0, [bc])
        c.update(idf=idf, idb=idb, onesf=onesf, onesb=onesb, utri=utri, sup=sup, negm=negm, negmb=negmb, epsc=epsc, onec=onec)
        self.c = c


def _silu_from(kb, out, in_, tmp, r, w, eng="dve"):
    kb.act(tmp, in_, AF.Sigmoid, r, w)
    kb.tt(eng, out, in_, tmp, ALU.mult, r, w)


def phase_mod(kb, T):
    c = kb.c
    kb.push()
    csb = kb.sb([128, 8], F32, "csb")
    cact = kb.sb([128, 8], F32, "cact")
    bcs = Buf()
    kb.dma(csb[:], T["c"].rearrange("o (p j) -> p (o j)", j=8), w=[bcs])
    kb.act(cact[:], csb[:], AF.Silu, [bcs], [bcs])
    NM = 6144 * 4 + 2048
    bmb = [Buf() for _ in range(3)]
    bmr = [Buf() for _ in range(3)]
    GW = 1024
    mw = [kb.sb([128, 8, GW], F32, "mw") for _ in range(3)]
    bmw = [Buf() for _ in range(3)]
    modb = [kb.sb([1, GW], F32, "modb") for _ in range(3)]
    modr = [kb.sb([1, GW], F32, "modr") for _ in range(3)]
    pm = [kb.ps([1, 512], F32, "pm") for _ in range(2)]
    bpm = [Buf(), Buf()]
    groups = []
    for l in range(4):
        wv = T["mod_w"][l].rearrange("(p j) n -> p j n", j=8)
        for n in range(6144 // GW):
            groups.append((wv[:, :, n * GW:(n + 1) * GW], T["mod_b"][l:l + 1, n * GW:(n + 1) * GW], l * 6144 + n * GW))
    wv = T["kv_mod_w"].rearrange("(p j) n -> p j n", j=8)
    for n in range(2048 // GW):
        groups.append((wv[:, :, n * GW:(n + 1) * GW], T["kv_mod_b"][0:1, n * GW:(n + 1) * GW], 24576 + n * GW))
    nm = 0
    for gi, (src, bsrc, col) in enumerate(groups):
        k3 = gi % 3
        kb.dma(mw[k3][:], src, w=[bmw[k3]])
        kb.dma(modb[k3][:], bsrc, w=[bmb[k3]])
        for hh in range(GW // 512):
            k2 = nm % 2
            nm += 1
            for j in range(8):
                kb.mm(pm[k2][:], cact[:, j:j + 1], mw[k3][:, j, hh * 512:(hh + 1) * 512], j == 0, j == 7, [bcs, bmw[k3]], [bpm[k2]])
            kb.tt("dve", modr[k3][:, hh * 512:(hh + 1) * 512], pm[k2][:], modb[k3][:, hh * 512:(hh + 1) * 512], ALU.add,
                  [bpm[k2], bmb[k3]], [bmr[k3]])
        kb.dma(T["modd"][0:1, col:col + GW], modr[k3][:], r=[bmr[k3]], w=[T["b_modd"]])
    kb.pop()


def load_mod(kb, T, layer, which):
    A = kb.sb([128, D], F32, "modA")
    Bt = kb.sb([128, D], F32, "modB")
    G = None
    gn = kb.sb([128, D], F32, "modg")
    b = Buf()
    if which == "kv":
        base = 24576
        gsrc = T["kv_norm_g"]
    else:
        base = layer * 6144 + (0 if which == "mix" else 3072)
        gsrc = T["norm_mix_g" if which == "mix" else "norm_ffn_g"][layer:layer + 1, :]
    md = T["modd"]
    kb.dma(Bt[:], md[0:1, base:base + 1024].partition_broadcast(128), r=[T["b_modd"]], w=[b])
    kb.dma(A[:], md[0:1, base + 1024:base + 2048].partition_broadcast(128), r=[T["b_modd"]], w=[b])
    kb.dma(gn[:], gsrc.partition_broadcast(128), w=[b])
    kb.stt("dve", A[:], A[:], 1.0, gn[:], ALU.add, ALU.mult, [b], [b])
    return A, Bt, G, b


class NormT:
    def __init__(self, kb, A, Bt, bmod, want32=False):
        self.kb = kb
        self.A, self.Bt, self.bmod = A, Bt, bmod
        self.xt = [kb.sb([128, D], F32, "xt") for _ in range(2)]
        self.ht = [kb.sb([128, D], F32, "ht") for _ in range(2)]
        self.junk = kb.sb([128, D], F32, "junk")
        self.ss = [kb.sb([128, 1], F32, "ss") for _ in range(2)]
        self.rs = [kb.sb([128, 1], F32, "rs") for _ in range(2)]
        self.want32 = want32
        self.pT = [kb.ps([128, 4, 128], F32, "pT") for _ in range(2)]
        if not want32:
            self.htb = [kb.sb([128, D], BF16, "htb") for _ in range(2)]
            self.pTb = [self.pT[0][:].bitcast(BF16), self.pT[1][:].bitcast(BF16)]
        self.b_xt = [Buf(), Buf()]
        self.b_ht = [Buf(), Buf()]
        self.b_junk = Buf()
        self.b_s = [Buf(), Buf()]
        self.b_pT = [Buf(), Buf()]
        self.n = 0

    def run(self, xsrc, bx, hT_dst, b_hT, hT32_dst=None, keep_x=False):
        st = self.run_a(xsrc, bx)
        return self.run_b(st, hT_dst, b_hT, hT32_dst)

    def run_a(self, xsrc, bx):
        kb = self.kb
        c = kb.c
        k = self.n % 2
        self.n += 1
        xt, ht = self.xt[k], self.ht[k]
        kb.dma(xt[:], xsrc, r=[bx], w=[self.b_xt[k]])
        kb.act(self.junk[:], xt[:], AF.Square, [self.b_xt[k]], [self.b_junk, self.b_s[k]], accum_out=self.ss[k][:])
        kb.act(self.rs[k][:], self.ss[k][:], AF.Sqrt, [self.b_s[k], kb.bc], [self.b_s[k]], scale=1.0 / D, bias=c["epsc"][:])
        kb.op("dve", lambda e: e.reciprocal(out=self.rs[k][:], in_=self.rs[k][:]), [self.b_s[k]], [self.b_s[k]])
        kb.stt("dve", ht[:], xt[:], self.rs[k][:, 0:1], self.A[:], ALU.mult, ALU.mult, [self.b_xt[k], self.b_s[k], self.bmod], [self.b_ht[k]])
        if self.want32:
            kb.tt("dve", ht[:], ht[:], self.Bt[:], ALU.add, [self.b_ht[k], self.bmod], [self.b_ht[k]])
        else:
            kb.tt("dve", self.htb[k][:], ht[:], self.Bt[:], ALU.add, [self.b_ht[k], self.bmod], [self.b_ht[k]])
        self.last_h = (ht, self.b_ht[k])
        return k

    def run_b(self, k, hT_dst, b_hT, hT32_dst=None):
        kb = self.kb
        c = kb.c
        xt, ht = self.xt[k], self.ht[k]
        if not self.want32:
            pv = self.pTb[0].rearrange("p a (b s) -> p (a b) s", s=128)
            for ch in range(8):
                kb.tr(pv[:, ch, :], self.htb[k][:, ch * 128:(ch + 1) * 128], c["idb"][:], [self.b_ht[k], kb.bc], [self.b_pT[0]])
            kb.copy("act", hT_dst[:, 0:4, :], pv[:, 0:4, :], [self.b_pT[0]], [b_hT])
            kb.copy("dve", hT_dst[:, 4:8, :], pv[:, 4:8, :], [self.b_pT[0]], [b_hT])
            return xt, self.b_xt[k]
        for half in range(2):
            for cc in range(4):
                ch = half * 4 + cc
                kb.tr(self.pT[half][:, cc, :], ht[:, ch * 128:(ch + 1) * 128], c["idf"][:], [self.b_ht[k], kb.bc], [self.b_pT[half]])
            eng = "act" if half == 0 else "dve"
            if hT_dst is not None:
                kb.copy(eng, hT_dst[:, half * 4:(half + 1) * 4, :], self.pT[half][:], [self.b_pT[half]], [b_hT])
            if hT32_dst is not None:
                eng2 = "dve" if half == 0 else "act"
                kb.copy(eng2, hT32_dst[:, half * 4:(half + 1) * 4, :], self.pT[half][:], [self.b_pT[half]], [b_hT])
        return xt, self.b_xt[k]


def pipeline(gens, extra=None):
    if os.environ.get("PIPE_SEQ"):
        for r, gf in enumerate(gens):
            if extra is not None:
                extra(r)
            for _ in gf():
                pass
        return
    live = []
    gi = 0
    r = 0
    while gi < len(gens) or live:
        if extra is not None:
            extra(r)
        nxt = []
        for g_ in live:
            try:
                next(g_)
                nxt.append(g_)
            except StopIteration:
                pass
        live = nxt
        if gi < len(gens):
            g_ = gens[gi]()
            gi += 1
            try:
                next(g_)
                live.append(g_)
            except StopIteration:
                pass
        r += 1


def load_w_bf16(kb, dst, src, K, N, b, kchunks=None):
    nk = K // 128
    for c in range(nk):
        n0 = 0
        while n0 < N:
            n1 = min(N, n0 + 2048)
            kb.dma(dst[:, c, n0:n1], src[c * 128:(c + 1) * 128, n0:n1], w=[b], q="pool")
            n0 = n1


def phase_gdn_proj(kb, T, l, xsrc, bx):
    c = kb.c
    kb.push()
    A, Bt, G, bmod = load_mod(kb, T, l, "mix")
    Win = kb.sb([128, 8, GPROJ], BF16, "Win")
    bW = Buf()
    load_w_bf16(kb, Win, T["gdn_w_in"][l], D, GPROJ, bW)
    cwr = kb.sb([96, 128], F32, "cwr")
    bcw = Buf()
    kb.dma(cwr[:], T["gdn_conv_w"][l].rearrange("k (c p) -> (k c) p", p=128), w=[bcw])
    cw = kb.sb([128, 96], F32, "cw")
    nega = kb.sb([128, 8], F32, "nega")
    dtb = kb.sb([128, 8], F32, "dtb")
    bsm = Buf()
    kb.dma(nega[:], T["gdn_a_log"][l:l + 1, :].partition_broadcast(128), w=[bsm])
    kb.dma(dtb[:], T["gdn_dt_bias"][l:l + 1, :].partition_broadcast(128), w=[bsm])
    kb.act(nega[:], nega[:], AF.Exp, [bsm], [bsm])
    kb.ts("dve", nega[:], nega[:], -1.0, None, ALU.mult, None, [bsm], [bsm])

    nt = NormT(kb, A, Bt, bmod)
    hTg = [kb.sb([128, 8, 512], BF16, "hTg") for _ in range(2)]
    b_hTg = [Buf(), Buf()]
    halo = kb.sb([128, 24, 4], BF16, "halo")
    b_halo = Buf()
    kb.memset("dve", halo[:], 0.0, [b_halo])
    pre = [kb.sb([128, 516], BF16, "pre") for _ in range(2)]
    b_pre = [Buf(), Buf()]
    dg = [kb.sb([128, 4, 128], BF16, "dg") for _ in range(2)]
    b_dg = [Buf(), Buf()]
    pq = [kb.ps([128, 512], F32, "pq") for _ in range(2)]
    b_pq = [Buf(), Buf()]
    psmall = kb.ps([128, 512], F32, "psmall")
    pcw = psmall[:, 32:128]
    kb.tr(pcw, cwr[:], c["idf"][0:96, 0:96], [bcw, kb.bc], [bcw])
    kb.copy("dve", cw[:], pcw, [bcw], [bcw])
    pc = kb.ps([128, 512], F32, "pc")
    b_pc = Buf()
    pn = kb.ps([128, 512], F32, "pn")
    b_pn = Buf()
    qst = [kb.sb([128, 4, 24, 128], BF16, "qst") for _ in range(2)]
    b_qst = [Buf(), Buf()]
    pab = psmall[:, 0:16]
    b_pab = Buf()
    zsg = [kb.sb([128, D], F32, "zsg") for _ in range(2)]
    zo = [kb.sb([128, D], BF16, "zo") for _ in range(2)]
    b_z = [Buf(), Buf()]
    ab = [kb.sb([128, 16], F32, "ab") for _ in range(2)]
    gbo = [kb.sb([128, 16], F32, "gbo") for _ in range(2)]
    b_ab = [Buf(), Buf()]
    pgc = psmall[:, 16:24]
    b_pgc = b_pab
    pc2 = [pc, kb.ps([128, 512], F32, "pc2")]
    b_pc2 = [b_pc, Buf()]
    sg3 = [kb.sb([128, 512], F32, "sg3") for _ in range(3)]
    qk3 = [kb.sb([128, 512], F32, "qk3") for _ in range(3)]
    sq3 = [kb.sb([128, 512], BF16, "sq3") for _ in range(3)]
    rn3 = [kb.sb([128, 512], F32, "rn3") for _ in range(3)]
    b_t3 = [Buf() for _ in range(3)]
    nz = 0

    def tile_small(g, t):
        nonlocal nz
        kg = g % 2
        i = g * 4 + t
        kz = nz % 2
        nz += 1
        for n in range(2):
            for cc in range(8):
                kb.mm(pq[n][:], hTg[kg][:, cc, t * 128:(t + 1) * 128], Win[:, cc, 3072 + n * 512:3072 + (n + 1) * 512],
                      cc == 0, cc == 7, [b_hTg[kg], bW], [b_pq[n]])
        for cc in range(8):
            kb.mm(pab, hTg[kg][:, cc, t * 128:(t + 1) * 128], Win[:, cc, 4096:4112], cc == 0, cc == 7, [b_hTg[kg], bW], [b_pab])
        for n in range(2):
            kb.act(zsg[kz][:, n * 512:(n + 1) * 512], pq[n][:], AF.Sigmoid, [b_pq[n]], [b_z[kz]])
            kb.tt("dve", zo[kz][:, n * 512:(n + 1) * 512], pq[n][:], zsg[kz][:, n * 512:(n + 1) * 512], ALU.mult, [b_pq[n], b_z[kz]], [b_z[kz]])
        kb.dma(T["sz_d"][i * 128:(i + 1) * 128, :], zo[kz][:], r=[b_z[kz]], w=[T["b_sz"][i]])
        kb.copy("dve", ab[kz][:], pab, [b_pab], [b_ab[kz]])
        kb.act(gbo[kz][:, 8:16], ab[kz][:, 8:16], AF.Sigmoid, [b_ab[kz]], [b_ab[kz]])
        kb.tt("dve", ab[kz][:, 0:8], ab[kz][:, 0:8], dtb[:], ALU.add, [b_ab[kz], bsm], [b_ab[kz]])
        kb.act(ab[kz][:, 0:8], ab[kz][:, 0:8], AF.Exp, [b_ab[kz]], [b_ab[kz]])
        kb.act(ab[kz][:, 0:8], ab[kz][:, 0:8], AF.Ln, [b_ab[kz], kb.bc], [b_ab[kz]], bias=c["onec"][:])
        kb.tt("dve", ab[kz][:, 0:8], ab[kz][:, 0:8], nega[:], ALU.mult, [b_ab[kz], bsm], [b_ab[kz]])
        kb.mm(pgc, c["utri"][:], ab[kz][:, 0:8], True, True, [b_ab[kz], kb.bc], [b_pgc])
        kb.copy("dve", gbo[kz][:, 0:8], pgc, [b_pgc], [b_ab[kz]])
        kb.dma(T["gb_d"][i * 128:(i + 1) * 128, :], gbo[kz][:], r=[b_ab[kz]], w=[T["b_gb"][i]])

    def chunk_gen(g, ch):
        kg = g % 2
        k2 = ch % 2
        k3 = ch % 3
        ov = qst[kg][:, :, ch, :]

        def gen():
            for cc in range(8):
                kb.mm(pq[k2][:], Win[:, cc, ch * 128:(ch + 1) * 128], hTg[kg][:, cc, :], cc == 0, cc == 7, [bW, b_hTg[kg]], [b_pq[k2]])
            yield
            kb.copy("dve", pre[k2][:, 0:4], halo[:, ch, :], [b_halo], [b_pre[k2]])
            kb.copy("act", pre[k2][:, 4:516], pq[k2][:], [b_pq[k2]], [b_pre[k2]])
            kb.copy("dve", halo[:, ch, :], pre[k2][:, 512:516], [b_pre[k2]], [b_halo])
            for kk in range(4):
                col = kk * 24 + ch
                kb.ts("dve", dg[k2][:, kk, :], c["idb"][:], cw[:, col:col + 1], None, ALU.mult, None, [bcw, kb.bc], [b_dg[k2]])
            yield
            for kk in range(4):
                kb.mm(pc2[k2][:], dg[k2][:, kk, :], pre[k2][:, 1 + kk:513 + kk], kk == 0, kk == 3, [b_dg[k2], b_pre[k2]], [b_pc2[k2]])
            yield
            kb.act(sg3[k3][:], pc2[k2][:], AF.Sigmoid, [b_pc2[k2]], [b_t3[k3]])
            if ch >= 16:
                kb.tt("dve", ov, pc2[k2][:].rearrange("p (t s) -> p t s", t=4), sg3[k3][:].rearrange("p (t s) -> p t s", t=4), ALU.mult,
                      [b_pc2[k2], b_t3[k3]], [b_qst[kg]])
                return
            kb.tt("dve", qk3[k3][:], pc2[k2][:], sg3[k3][:], ALU.mult, [b_pc2[k2], b_t3[k3]], [b_t3[k3]])
            kb.tt("dve", sq3[k3][:], qk3[k3][:], qk3[k3][:], ALU.mult, [b_t3[k3]], [b_t3[k3]])
            yield
            kb.mm(pn[:], c["onesb"][:], sq3[k3][:], True, True, [b_t3[k3], kb.bc], [b_pn])
            yield
            kb.act(rn3[k3][:], pn[:], AF.Sqrt, [b_pn, kb.bc], [b_t3[k3]], bias=c["epsc"][:])
            kb.op("dve", lambda e: e.reciprocal(out=rn3[k3][:], in_=rn3[k3][:]), [b_t3[k3]], [b_t3[k3]])
            qs = (128.0 ** -0.5) if ch < 8 else 1.0
            kb.stt("dve", ov, qk3[k3][:].rearrange("p (t s) -> p t s", t=4), qs, rn3[k3][:].rearrange("p (t s) -> p t s", t=4),
                   ALU.mult, ALU.mult, [b_t3[k3]], [b_qst[kg]])
        return gen

    NG = S // 512
    for t in range(4):
        nt.run(xsrc[t * 128:(t + 1) * 128, :], bx[t], hTg[0][:, :, t * 128:(t + 1) * 128], b_hTg[0])
    for g in range(NG):
        kg = g % 2
        for t in range(4):
            tile_small(g, t)
        pend = {}

        def extra(r, g=g):
            if g + 1 >= NG:
                return
            t, ph = divmod(r, 6)
            if t < 4 and ph == 1:
                i = (g + 1) * 4 + t
                pend[t] = nt.run_a(xsrc[i * 128:(i + 1) * 128, :], bx[i])
            if t < 4 and ph == 4:
                kn = (g + 1) % 2
                nt.run_b(pend[t], hTg[kn][:, :, t * 128:(t + 1) * 128], b_hTg[kn])

        pipeline([chunk_gen(g, ch) for ch in range(24)], extra)
        for t in range(4):
            i = g * 4 + t
            kb.dma(T["qkv_d"][i], qst[kg][:, t, :, :].rearrange("p c s -> p (c s)"), r=[b_qst[kg]], w=[T["b_qkv"][i]])
    kb.pop()


SOLVE_DT = F32
F32R = mybir.dt.float32r
SOLVE_R = False


def b3(ap, shape):
    return ap.to_broadcast(shape)


def phase_gdn_delta(kb, T, l, xsrc, bx, xdst, bxd):
    c = kb.c
    SD = SOLVE_DT
    kb.push()
    H3 = [128, 8, 128]
    G = kb.sb([128, D], F32, "G")
    bG = Buf()
    base = l * 6144 + 2048
    kb.dma(G[:], T["modd"][0:1, base:base + 1024].partition_broadcast(128), r=[T["b_modd"]], w=[bG])
    gon = kb.sb([128, 128], F32, "gon")
    kb.dma(gon[:], T["gdn_out_norm_g"][l:l + 1, :].partition_broadcast(128), w=[bG])
    Wout = kb.sb([128, 8, D], BF16, "Wout")
    bW = Buf()
    load_w_bf16(kb, Wout, T["gdn_w_out"][l], D, D, bW)
    S32 = kb.sb(H3, F32, "S32")
    Sb = kb.sb(H3, BF16, "Sb")
    bS32 = Buf()
    bSb = Buf()
    kb.memset("dve", S32[:], 0.0, [bS32])
    kb.memset("dve", Sb[:], 0.0, [bSb])
    idf3 = c["idf"][:].unsqueeze(1).to_broadcast(H3)
    negm3 = c["negm"][:].unsqueeze(1).to_broadcast(H3)
    sup3 = c["sup"][:].unsqueeze(1).to_broadcast(H3)
    gon3 = gon[:].unsqueeze(1).to_broadcast(H3)

    R = [kb.ps(H3, F32, "R") for _ in range(3)]
    bR = [Buf() for _ in range(3)]
    Rb = kb.ps([128, 2, 8, 128], BF16, "Rb")
    bRb = [Buf(), Buf()]

    qkvt = [kb.sb([128, 24, 128], BF16, "qkvt") for _ in range(2)]
    b_qkvt = [Buf(), Buf()]
    gb = [kb.sb([128, 16], F32, "gb") for _ in range(2)]
    glast = [kb.sb([128, 8], F32, "glast") for _ in range(2)]
    b_gb = [Buf(), Buf()]
    szt = [kb.sb([128, D], BF16, "szt") for _ in range(2)]
    b_sz = [Buf(), Buf()]
    xt = [kb.sb([128, D], F32, "xt") for _ in range(2)]
    b_xt = [Buf(), Buf()]
    sc = kb.sb([128, 5, 8], F32, "sc")
    b_sc = Buf()

    def t3(dt, nm):
        return kb.sb(H3, dt, nm), Buf()
    kb_tm, b_kb = t3(BF16, "kb_tm")
    kbe_tm, b_kbe = t3(BF16, "kbe_tm")
    kdec_tm, b_kdec = t3(BF16, "kdec_tm")
    vb_tm, b_vb = t3(BF16, "vb_tm")
    diagc, b_diag = t3(F32, "diagc")
    tmpf, b_tmpf = t3(F32, "tmpf")
    DT, b_DT = t3(F32, "DT")
    kbT, b_kbT = t3(BF16, "kbT")
    SDA = F32R if (SOLVE_R and SD == F32) else SD
    AT, b_AT = t3(SDA, "AT")
    Am, b_A = t3(SDA, "Am")
    Mp = [t3(SDA, "Mp") for _ in range(2)]
    MTp = [t3(SDA, "MTp") for _ in range(2)]
    Xp = [t3(SDA, "Xp") for _ in range(2)]
    Xb, b_Xb = t3(BF16, "Xb")
    wT, b_wT = t3(BF16, "wT")
    u, b_u = t3(F32, "u")
    vnew, b_vnew = t3(BF16, "vnew")
    attnT, b_attn = t3(BF16, "attnT")
    tmpo, b_tmpo = t3(F32, "tmpo")
    o, b_o = t3(F32, "o")
    sqo, b_sqo = t3(F32, "sqo")
    og = kb.sb([128, D], BF16, "og")
    b_og = Buf()
    ogT, b_ogT = t3(BF16, "ogT")
    ssq = kb.sb([128, 2, 8], F32, "ssq")
    b_ssq = Buf()
    yt = kb.sb([128, D], F32, "yt")
    b_yt = Buf()
    idS = c["idf"] if SD == F32 else c["idb"]

    def loads(i):
        k = i % 2
        kb.dma(qkvt[k][:].rearrange("p c s -> p (c s)"), T["qkv_d"][i], r=[T["b_qkv"][i]], w=[b_qkvt[k]])
        kb.dma(gb[k][:], T["gb_d"][i * 128:(i + 1) * 128, :], r=[T["b_gb"][i]], w=[b_gb[k]])
        kb.dma(glast[k][:], T["gb_d"][i * 128 + 127:i * 128 + 128, 0:8].partition_broadcast(128), r=[T["b_gb"][i]], w=[b_gb[k]])
        kb.dma(szt[k][:], T["sz_d"][i * 128:(i + 1) * 128, :], r=[T["b_sz"][i]], w=[b_sz[k]])
        kb.dma(xt[k][:], xsrc[i * 128:(i + 1) * 128, :], r=[bx[i]], w=[b_xt[k]])

    loads(0)
    for i in range(NT):
        k = i % 2
        if i + 1 < NT:
            loads(i + 1)
        q3 = qkvt[k][:, 0:8, :]
        k3 = qkvt[k][:, 8:16, :]
        v3 = qkvt[k][:, 16:24, :]
        gc = gb[k][:, 0:8]
        beta = gb[k][:, 8:16]
        egc, bege, dk, egl = sc[:, 0, :], sc[:, 1, :], sc[:, 2, :], sc[:, 3, :]
        rdeps = [b_gb[k]]
        kb.act(egc, gc, AF.Exp, rdeps, [b_sc])
        kb.tt("dve", bege, egc, beta, ALU.mult, rdeps + [b_sc], [b_sc])
        kb.tt("dve", dk, glast[k][:], gc, ALU.subtract, rdeps, [b_sc])
        kb.act(dk, dk, AF.Exp, [b_sc], [b_sc])
        kb.act(egl, glast[k][:], AF.Exp, rdeps, [b_sc])
        for h in range(8):
            kb.tr(Rb[:, 0, h, :], qkvt[k][:, 8 + h, :], c["idb"][:], [b_qkvt[k], kb.bc], [bRb[0]])
        for h in range(8):
            kb.tr(Rb[:, 1, h, :], qkvt[k][:, 16 + h, :], c["idb"][:], [b_qkvt[k], kb.bc], [bRb[1]])
        kb.tt("dve", kb_tm[:], Rb[:, 0, :, :], beta.unsqueeze(2).to_broadcast(H3), ALU.mult, [bRb[0], b_gb[k]], [b_kb])
        kb.tt("dve", kbe_tm[:], Rb[:, 0, :, :], bege.unsqueeze(2).to_broadcast(H3), ALU.mult, [bRb[0], b_sc], [b_kbe])
        kb.tt("dve", kdec_tm[:], Rb[:, 0, :, :], dk.unsqueeze(2).to_broadcast(H3), ALU.mult, [bRb[0], b_sc], [b_kdec])
        kb.tt("dve", vb_tm[:], Rb[:, 1, :, :], beta.unsqueeze(2).to_broadcast(H3), ALU.mult, [bRb[1], b_gb[k]], [b_vb])
        kb.tt("dve", diagc[:], idf3, gc.unsqueeze(2).to_broadcast(H3), ALU.mult, [kb.bc, b_gb[k]], [b_diag])
        for hh in range(2):
            kb.mm(R[0][:, hh * 4:(hh + 1) * 4, :], c["onesf"][:], diagc[:, hh * 4:(hh + 1) * 4, :], True, True, [kb.bc, b_diag], [bR[0]])
        kb.tt("dve", tmpf[:], R[0][:], gc.unsqueeze(2).to_broadcast(H3), ALU.subtract, [bR[0], b_gb[k]], [b_tmpf])
        kb.tt("dve", tmpf[:], tmpf[:], negm3, ALU.add, [b_tmpf, kb.bc], [b_tmpf])
        kb.act(DT[:], tmpf[:], AF.Exp, [b_tmpf], [b_DT])
        for h in range(8):
            kb.tr(Rb[:, 0, h, :], kb_tm[:, h, :], c["idb"][:], [b_kb, kb.bc], [bRb[0]])
        kb.copy("act", kbT[:], Rb[:, 0, :, :], [bRb[0]], [b_kbT])
        for h in range(8):
            kb.mm(R[1][:, h, :], qkvt[k][:, 8 + h, :], kbT[:, h, :], True, True, [b_qkvt[k], b_kbT], [bR[1]])
        kb.tt("dve", tmpf[:], R[1][:], DT[:], ALU.mult, [bR[1], b_DT], [b_tmpf])
        kb.tt("dve", AT[:], tmpf[:], sup3, ALU.mult, [b_tmpf, kb.bc], [b_AT])
        if SD == F32:
            for h in range(8):
                kb.tr(R[0][:, h, :], AT[:, h, :].bitcast(F32) if SDA == F32R else AT[:, h, :], idS[:], [b_AT, kb.bc], [bR[0]])
            kb.copy("act", Am[:], R[0][:], [bR[0]], [b_A])
        else:
            for h in range(8):
                kb.tr(Rb[:, 1, h, :], AT[:, h, :], idS[:], [b_AT, kb.bc], [bRb[1]])
            kb.copy("act", Am[:], Rb[:, 1, :, :], [bRb[1]], [b_A])
        X, bX = Xp[0]
        kb.tt("dve", X[:], idf3, AT[:], ALU.subtract, [kb.bc, b_AT], [bX])
        HS = [slice(0, 4), slice(4, 8)]
        hbR = [[Buf(), Buf()] for _ in range(3)]
        for r_ in range(3):
            for hf in range(2):
                hbR[r_][hf].last_w = bR[r_].last_w
                hbR[r_][hf].readers = dict(bR[r_].readers)
        hM = [Buf(), Buf()]
        hMT = [Buf(), Buf()]
        hX = [Buf(), Buf()]
        for hf in range(2):
            hM[hf].last_w = b_A.last_w
            hMT[hf].last_w = b_AT.last_w
            hX[hf].last_w = bX.last_w
        M, MT = Am, AT
        all_half_bufs = []
        for it in range(6):
            Mn, _bMn = Mp[it % 2]
            MTn, _bMTn = MTp[it % 2]
            Xn, _bXn = Xp[(it + 1) % 2]
            hMn = [Buf(), Buf()]
            hMTn = [Buf(), Buf()]
            hXn = [Buf(), Buf()]
            for hf in range(2):
                hMn[hf].readers = dict(_bMn.readers); hMn[hf].last_w = _bMn.last_w
                hMTn[hf].readers = dict(_bMTn.readers); hMTn[hf].last_w = _bMTn.last_w
                hXn[hf].readers = dict(_bXn.readers); hXn[hf].last_w = _bXn.last_w
            for hf in range(2):
                hs = HS[hf]
                for h in range(hs.start, hs.stop):
                    kb.mm(R[0][:, h, :], MT[:, h, :], M[:, h, :], True, True, [hMT[hf], hM[hf]], [hbR[0][hf]])
                if it < 5:
                    for h in range(hs.start, hs.stop):
                        kb.mm(R[1][:, h, :], M[:, h, :], MT[:, h, :], True, True, [hMT[hf], hM[hf]], [hbR[1][hf]])
                kb.copy("act", Mn[:, hs, :], R[0][:, hs, :], [hbR[0][hf]], [hMn[hf]])
                if it < 5:
                    kb.copy("dve", MTn[:, hs, :], R[1][:, hs, :], [hbR[1][hf]], [hMTn[hf]])
            for hf in range(2):
                hs = HS[hf]
                for h in range(hs.start, hs.stop):
                    kb.mm(R[2][:, h, :], Mn[:, h, :], X[:, h, :], True, True, [hMn[hf], hX[hf]], [hbR[2][hf]])
                kb.tt("dve", Xn[:, hs, :], R[2][:, hs, :], X[:, hs, :], ALU.add, [hbR[2][hf], hX[hf]], [hXn[hf]])
            for whole, halves in ((_bMn, hMn), (_bMTn, hMTn), (_bXn, hXn)):
                pass
            X = Xn
            M, MT = Mn, MTn
            hM, hMT, hX = hMn, hMTn, hXn
            all_half_bufs.append((hMn, hMTn, hXn))
        bX_halves = hX
        bR_half = hbR
        if SD == F32:
            kb.copy("dve", Xb[:], X[:], list(bX_halves), [b_Xb])
            XB, bXB = Xb, b_Xb
        else:
            XB, bXB = X, bX
        for h in range(8):
            kb.mm(R[0][:, h, :], kbe_tm[:, h, :], XB[:, h, :], True, True, [b_kbe, bXB], [bR[0]] + bR_half[0])
        for h in range(8):
            kb.mm(R[1][:, h, :], XB[:, h, :], vb_tm[:, h, :], True, True, [b_vb, bXB], [bR[1]] + bR_half[1])
        kb.copy("act", wT[:], R[0][:], [bR[0]], [b_wT])
        kb.copy("dve", u[:], R[1][:], [bR[1]], [b_u])
        for h in range(8):
            kb.mm(R[2][:, h, :], wT[:, h, :], Sb[:, h, :], True, True, [b_wT, bSb], [bR[2]] + bR_half[2])
        for h in range(8):
            kb.mm(R[0][:, h, :], qkvt[k][:, h, :], Sb[:, h, :], True, True, [b_qkvt[k], bSb], [bR[0]])
        for h in range(8):
            kb.mm(R[1][:, h, :], qkvt[k][:, 8 + h, :], qkvt[k][:, h, :], True, True, [b_qkvt[k]], [bR[1]])
        kb.tt("dve", vnew[:], u[:], R[2][:], ALU.subtract, [b_u, bR[2]], [b_vnew])
        kb.tt("dve", tmpo[:], R[0][:], egc.unsqueeze(2).to_broadcast(H3), ALU.mult, [bR[0], b_sc], [b_tmpo])
        kb.tt("dve", attnT[:], R[1][:], DT[:], ALU.mult, [bR[1], b_DT], [b_attn])
        for h in range(8):
            kb.mm(R[2][:, h, :], kdec_tm[:, h, :], vnew[:, h, :], True, True, [b_kdec, b_vnew], [bR[2]])
        for h in range(8):
            kb.mm(R[1][:, h, :], attnT[:, h, :], vnew[:, h, :], True, True, [b_attn, b_vnew], [bR[1]])
        kb.tt("dve", S32[:], S32[:], egl.unsqueeze(2).to_broadcast(H3), ALU.mult, [bS32, b_sc], [bS32])
        kb.tt("dve", S32[:], S32[:], R[2][:], ALU.add, [bS32, bR[2]], [bS32])
        kb.copy("act", Sb[:], S32[:], [bS32], [bSb])
        kb.tt("dve", o[:], tmpo[:], R[1][:], ALU.add, [b_tmpo, bR[1]], [b_o])
        kb.tt("dve", sqo[:], o[:], o[:], ALU.mult, [b_o], [b_sqo])
        kb.op("dve", lambda e: e.tensor_reduce(out=ssq[:, 0, :], in_=sqo[:], axis=AX.X, op=ALU.add), [b_sqo], [b_ssq])
        kb.act(ssq[:, 1, :], ssq[:, 0, :], AF.Sqrt, [b_ssq, kb.bc], [b_ssq], scale=1.0 / 128, bias=c["epsc"][:])
        kb.op("dve", lambda e: e.reciprocal(out=ssq[:, 1, :], in_=ssq[:, 1, :]), [b_ssq], [b_ssq])
        kb.tt("dve", sqo[:], o[:], ssq[:, 1, :].unsqueeze(2).to_broadcast(H3), ALU.mult, [b_o, b_ssq], [b_sqo])
        kb.tt("dve", sqo[:], sqo[:], gon3, ALU.mult, [b_sqo, bG], [b_sqo])
        kb.tt("dve", og[:], sqo[:].rearrange("p h d -> p (h d)"), szt[k][:], ALU.mult, [b_sqo, b_sz[k]], [b_og])
        for cc in range(8):
            kb.tr(Rb[:, 1, cc, :], og[:, cc * 128:(cc + 1) * 128], c["idb"][:], [b_og, kb.bc], [bRb[1]])
        kb.copy("act", ogT[:], Rb[:, 1, :, :], [bRb[1]], [b_ogT])
        R0f = R[0][:].rearrange("p h d -> p (h d)")
        for n in range(2):
            for cc in range(8):
                kb.mm(R0f[:, n * 512:(n + 1) * 512], ogT[:, cc, :], Wout[:, cc, n * 512:(n + 1) * 512], cc == 0, cc == 7, [b_ogT, bW], [bR[0]])
        kb.tt("dve", yt[:], R0f, G[:], ALU.mult, [bR[0], bG], [b_yt])
        kb.tt("dve", yt[:], yt[:], xt[k][:], ALU.add, [b_yt, b_xt[k]], [b_yt])
        kb.dma(xdst[i * 128:(i + 1) * 128, :], yt[:], r=[b_yt], w=[bxd[i]])
    kb.pop()


OOB_IDX = 1 << 28
_REGS = {}


def breg(e, val):
    key = (id(e), val)
    if key not in _REGS:
        _REGS[key] = e.to_reg(val)
    return _REGS[key]


def phase_moe(kb, T, l, xsrc, bx, xdst, bxd):
    c = kb.c
    kb.push()
    wts = kb.sb([128, NT, 2], F32, "wts")
    b_wts = Buf()
    kb.push()
    A, Bt, G_unused, bmod = load_mod(kb, T, l, "ffn")
    nt = NormT(kb, A, Bt, bmod, want32=True)
    Wr = kb.sb([128, 8, 36], F32, "Wr")
    bWr = Buf()
    kb.dma(Wr[:, :, 0:4], T["moe_w_group"][l].rearrange("(c p) n -> p c n", p=128), w=[bWr])
    kb.dma(Wr[:, :, 4:36], T["moe_w_expert"][l].rearrange("(c p) n -> p c n", p=128), w=[bWr])
    rb = kb.sb([128, 36], F32, "rb")
    kb.dma(rb[:, 0:4], T["moe_b_group"][l:l + 1, :].partition_broadcast(128), w=[bWr])
    kb.dma(rb[:, 4:36], T["moe_b_expert"][l:l + 1, :].partition_broadcast(128), w=[bWr])
    egrp = kb.sb([128, 32], F32, "egrp")
    eio = kb.sb([128, 32], F32, "eio")
    eioi = kb.sb([128, 32], I32, "eioi")
    bce = Buf()
    for g in range(4):
        kb.memset("dve", egrp[:, g * 8:(g + 1) * 8], float(g), [bce])
    kb.op("pool", lambda e: e.iota(eioi[:], pattern=[[1, 32]], base=0, channel_multiplier=0), (), [bce])
    kb.copy("dve", eio[:], eioi[:], [bce], [bce])
    sent = kb.sb([128, NE * CAP * 2 // 128], I32, "sent")
    kb.memset("dve", sent[:], OOB_IDX, [bce])
    kb.dma(T["rowasg"].rearrange("(p j) o -> p (j o)", p=128), sent[:], r=[bce], w=[T["b_rowasg"]])
    cnt = kb.sb([128, 32], F32, "cnt")
    b_cnt = Buf()
    kb.memset("dve", cnt[:], 0.0, [b_cnt])
    hT32 = [kb.sb([128, 8, 128], F32, "hT32") for _ in range(2)]
    b_hT32 = [Buf(), Buf()]
    hb = [kb.sb([128, D], BF16, "hb") for _ in range(2)]
    b_hb = [Buf(), Buf()]
    pl = kb.ps([128, 512], F32, "pl")
    b_pl = Buf()
    pp = kb.ps([128, 512], F32, "pp")
    b_pp = Buf()
    W = 160
    sm = [kb.sb([128, W], F32, "sm") for _ in range(2)]
    smu = [kb.sb([128, 16], U32, "smu") for _ in range(2)]
    smi = [kb.sb([128, 4], I32, "smi") for _ in range(2)]
    aid_all = kb.sb([128, NT, 2, 2], I32, "aid_all")
    kb.op("pool", lambda e: e.iota(aid_all[:], pattern=[[128, NT], [S, 2], [0, 2]], base=0, channel_multiplier=1), (), [bce])
    b_sm = [Buf(), Buf()]
    recs = []
    for i in range(NT):
        k = i % 2
        kb.P.rec = []
        nt.run(xsrc[i * 128:(i + 1) * 128, :], bx[i], None, b_hT32[k], hT32_dst=hT32[k])
        ht, b_ht = nt.last_h
        kb.copy("dve", hb[k][:], ht[:], [b_ht], [b_hb[k]])
        kb.dma(T["hd"][i * 128:(i + 1) * 128, :], hb[k][:], r=[b_hb[k]], w=[T["b_hd"]])
        kb.dma(T["hd"][S + i * 128:S + (i + 1) * 128, :], hb[k][:], r=[b_hb[k]], w=[T["b_hd"]])
        for cc in range(8):
            kb.mm(pl[:, 0:36], hT32[k][:, cc, :], Wr[:, cc, :], cc == 0, cc == 7, [b_hT32[k], bWr], [b_pl])
        m = sm[k]
        B = [b_sm[k]]
        lg8 = m[:, 0:8]
        le = m[:, 8:40]
        gmax = m[:, 40:48]
        lem = m[:, 48:80]
        emax = m[:, 80:88]
        msk = m[:, 88:120]
        sc_ = m[:, 120:160]
        kb.memset("dve", m[:, 4:8], -1e30, B)
        kb.tt("dve", m[:, 0:4], pl[:, 0:4], rb[:, 0:4], ALU.add, [b_pl, bWr], B)
        kb.tt("dve", le, pl[:, 4:36], rb[:, 4:36], ALU.add, [b_pl, bWr], B)
        kb.op("dve", lambda e, lg8=lg8, gmax=gmax: e.max(out=gmax, in_=lg8), B, B)
        kb.op("dve", lambda e, k=k, lg8=lg8, gmax=gmax: e.max_index(out=smu[k][:, 0:8], in_max=gmax, in_values=lg8), B, B)
        kb.ts("dve", sc_[:, 0:1], gmax[:, 0:1], -1.0, None, ALU.mult, None, B, B)
        kb.act(sc_[:, 8:12], m[:, 0:4], AF.Exp, B, B, bias=sc_[:, 0:1], accum_out=sc_[:, 1:2])
        kb.op("dve", lambda e, sc_=sc_: e.reciprocal(out=sc_[:, 2:3], in_=sc_[:, 1:2]), B, B)
        kb.copy("dve", sc_[:, 3:4], smu[k][:, 0:1], B, B)
        kb.ts("dve", msk, egrp[:], sc_[:, 3:4], None, ALU.is_equal, None, B + [bce], B)
        kb.ts("dve", msk, msk, 1e30, -1e30, ALU.mult, ALU.add, B, B)
        kb.tt("dve", lem, le, msk, ALU.add, B, B)
        kb.op("dve", lambda e, lem=lem, emax=emax: e.max(out=emax, in_=lem), B, B)
        kb.op("dve", lambda e, k=k, lem=lem, emax=emax: e.max_index(out=smu[k][:, 8:16], in_max=emax, in_values=lem), B, B)
        kb.tt("dve", sc_[:, 4:5], emax[:, 0:1], emax[:, 1:2], ALU.subtract, B, B)
        kb.act(sc_[:, 5:6], sc_[:, 4:5], AF.Sigmoid, B, B)
        kb.ts("dve", sc_[:, 6:7], sc_[:, 5:6], -1.0, 1.0, ALU.mult, ALU.add, B, B)
        kb.ts("dve", wts[:, i, :], sc_[:, 5:7], sc_[:, 2:3], None, ALU.mult, None, B, [b_wts])
        kb.copy("dve", sc_[:, 12:14], smu[k][:, 8:10], B, B)
        M0 = m[:, 88:120]
        M1 = m[:, 48:80]
        kb.ts("dve", M0, eio[:], sc_[:, 12:13], None, ALU.is_equal, None, B + [bce], B)
        kb.ts("dve", M1, eio[:], sc_[:, 13:14], None, ALU.is_equal, None, B + [bce], B)
        Mb = m[:, 8:40]
        kb.tt("dve", Mb, M0, M1, ALU.add, B, B)
        kb.mm(pp[:, 0:32], c["sup"][:], Mb, True, True, B + [kb.bc], [b_pp])
        kb.mm(pp[:, 32:64], c["onesf"][:], Mb, True, True, B + [kb.bc], [b_pp])
        rbase = m[:, 8:40]
        kb.tt("dve", rbase, pp[:, 0:32], cnt[:], ALU.add, [b_pp, b_cnt], B)
        kb.tt("dve", cnt[:], cnt[:], pp[:, 32:64], ALU.add, [b_pp, b_cnt], [b_cnt])
        kb.tt("dve", M0, M0, rbase, ALU.mult, B, B)
        kb.tt("dve", M1, M1, rbase, ALU.mult, B, B)
        kb.op("dve", lambda e, M0=M0, sc_=sc_: e.tensor_reduce(out=sc_[:, 14:15], in_=M0, axis=AX.X, op=ALU.add), B, B)
        kb.op("dve", lambda e, M1=M1, sc_=sc_: e.tensor_reduce(out=sc_[:, 15:16], in_=M1, axis=AX.X, op=ALU.add), B, B)
        kb.stt("dve", sc_[:, 16:18], sc_[:, 12:14], float(CAP), sc_[:, 14:16], ALU.mult, ALU.add, B, B)
        kb.ts("dve", sc_[:, 18:20], sc_[:, 14:16], float(CAP), None, ALU.is_ge, None, B, B)
        kb.stt("dve", sc_[:, 16:18], sc_[:, 18:20], float(OOB_IDX), sc_[:, 16:18], ALU.mult, ALU.add, B, B)
        kb.copy("dve", smi[k][:, 0:2], sc_[:, 16:18], B, B)
        for kk in range(2):
            kb.P.dma("pool", lambda e, k=k, kk=kk, i=i: e.indirect_dma_start(
                out=T["rowasg"], out_offset=bass.IndirectOffsetOnAxis(ap=smi[k][:, kk:kk + 1], axis=0),
                in_=aid_all[:, i, kk, :], in_offset=None, bounds_check=breg(e, NE * CAP - 1), oob_is_err=False), B + [bce], [T["b_rowasg"]])
        recs.append(kb.P.rec)
        kb.P.rec = None
        if len(recs) == 2:
            kb.P.replay_interleaved(recs, skew=6)
            recs = []
    cnti = kb.sb([1, 32], I32, "cnti")
    kb.copy("dve", cnti[:], cnt[0:1, :], [b_cnt], [b_cnt])
    kb.dma(T["cnt_d"], cnti[:], r=[b_cnt], w=[T["b_cntd"]])
    kb.pop()
    kb.push()
    cregs = {}

    def load_cnt(e):
        for en in Prog.ENGS:
            def ld(eo, e=e):
                key = (id(eo), e % 2)
                if key not in cregs:
                    cregs[key] = eo.alloc_register("moecnt%d_%d_%d" % (l, e % 2, len(cregs)))
                return eo.reg_load(cregs[key], T["cnt_d"][0:1, e:e + 1])
            kb.P.op(en, ld, [T["b_cntd"]], ())

    def cond_for(e, s_):
        def cond(eo):
            return eo.If_cmp(cregs[(id(eo), e % 2)], s_ * 128, "IS_GT")
        return cond
    Wg = [kb.sb([128, 8, DE], BF16, "Wg") for _ in range(2)]
    Wu = [kb.sb([128, 8, DE], BF16, "Wu") for _ in range(2)]
    Wd = [kb.sb([128, 4, D], BF16, "Wd") for _ in range(2)]
    b_Wg = [Buf(), Buf()]
    b_Wu = [Buf(), Buf()]
    b_Wd = [Buf(), Buf()]
    idx = [kb.sb([128, 2], I32, "idx") for _ in range(3)]
    b_idx = [Buf() for _ in range(3)]
    Xg = [kb.sb([128, D], BF16, "Xg") for _ in range(3)]
    b_Xg = [Buf() for _ in range(3)]
    for j in range(3):
        kb.memset("dve", Xg[j][:], 0.0, [b_Xg[j]])
    XgT = [kb.sb([128, 8, 128], BF16, "XgT") for _ in range(2)]
    b_XgT = [Buf(), Buf()]
    pT = kb.ps([128, 8, 128], BF16, "pT")
    b_pT = Buf()
    pg = kb.ps([128, 512], F32, "pg")
    b_pg = Buf()
    pu = kb.ps([128, 512], F32, "pu")
    b_pu = Buf()
    py = [kb.ps([128, 512], F32, "py") for _ in range(2)]
    b_py = [Buf(), Buf()]
    sgm = [kb.sb([128, 512], F32, "sgm") for _ in range(2)]
    av = [kb.sb([128, 512], BF16, "av") for _ in range(2)]
    b_av = [Buf(), Buf()]
    aT = [kb.sb([128, 4, 128], BF16, "aT") for _ in range(2)]
    b_aT = [Buf(), Buf()]
    yr = [kb.sb([128, D], F32, "yr") for _ in range(2)]
    b_yr = [Buf(), Buf()]

    stg = [kb.sb([128, 8, DE], F32, "stg_g"), kb.sb([128, 8, DE], F32, "stg_u"), kb.sb([128, 4, D], F32, "stg_d")]
    b_stg = [[Buf(), Buf(), Buf()], [Buf(), Buf(), Buf()]]

    def load_expert_dma(e):
        k = e % 2
        st = stg
        kb.dma(st[0][:], T["moe_w_gate"][l, e].rearrange("(c p) n -> p c n", p=128), w=[b_stg[0][0]])
        kb.dma(st[1][:], T["moe_w_up"][l, e].rearrange("(c p) n -> p c n", p=128), w=[b_stg[0][1]])
        kb.dma(st[2][:], T["moe_w_down"][l, e].rearrange("(c p) n -> p c n", p=128), w=[b_stg[0][2]])

    def cast_expert(e):
        k = e % 2
        st = stg
        kb.copy("act", Wg[k][:], st[0][:], [b_stg[0][0]], [b_Wg[k]])
        kb.copy("dve", Wu[k][:], st[1][:], [b_stg[0][1]], [b_Wu[k]])
        kb.copy("dve", Wd[k][:], st[2][:], [b_stg[0][2]], [b_Wd[k]])

    def gather(n):
        e, s_ = divmod(n, NSLOT)
        j = n % 3
        r0 = e * CAP + s_ * 128
        kb.dma(idx[j][:], T["rowasg"][r0:r0 + 128, :], r=[T["b_rowasg"]], w=[b_idx[j]])
        kb.P.dma("pool", lambda en, j=j: en.indirect_dma_start(
            out=Xg[j][:], out_offset=None, in_=T["hd"], in_offset=bass.IndirectOffsetOnAxis(ap=idx[j][:, 0:1], axis=0),
            bounds_check=breg(en, 2 * S - 1), oob_is_err=False), [b_idx[j], T["b_hd"]], [b_Xg[j]])

    load_expert_dma(0)
    cast_expert(0)
    load_cnt(0)
    kb.P.begin_guard(cond_for(0, 0))
    gather(0)
    kb.P.end_guard()
    ntot = NE * NSLOT
    for n in range(ntot):
        e, s_ = divmod(n, NSLOT)
        kw = e % 2
        j = n % 3
        k2 = n % 2
        if s_ == 0 and e + 1 < NE:
            load_cnt(e + 1)
        if s_ == 0 and e + 1 < NE:
            load_expert_dma(e + 1)
        if n + 1 < ntot:
            e1, s1 = divmod(n + 1, NSLOT)
            kb.P.begin_guard(cond_for(e1, s1))
            gather(n + 1)
            kb.P.end_guard()
        kb.P.begin_guard(cond_for(e, s_))
        for cc in range(8):
            kb.tr(pT[:, cc, :], Xg[j][:, cc * 128:(cc + 1) * 128], c["idb"][:], [b_Xg[j], kb.bc], [b_pT])
        kb.copy("act", XgT[k2][:], pT[:], [b_pT], [b_XgT[k2]])
        for cc in range(8):
            kb.mm(pg[:], XgT[k2][:, cc, :], Wg[kw][:, cc, :], cc == 0, cc == 7, [b_XgT[k2], b_Wg[kw]], [b_pg])
        for cc in range(8):
            kb.mm(pu[:], XgT[k2][:, cc, :], Wu[kw][:, cc, :], cc == 0, cc == 7, [b_XgT[k2], b_Wu[kw]], [b_pu])
        kb.act(sgm[k2][:], pg[:], AF.Sigmoid, [b_pg], [b_av[k2]])
        kb.tt("dve", sgm[k2][:], sgm[k2][:], pg[:], ALU.mult, [b_pg, b_av[k2]], [b_av[k2]])
        kb.tt("dve", av[k2][:], sgm[k2][:], pu[:], ALU.mult, [b_pu, b_av[k2]], [b_av[k2]])
        for cc in range(4):
            kb.tr(pT[:, cc, :], av[k2][:, cc * 128:(cc + 1) * 128], c["idb"][:], [b_av[k2], kb.bc], [b_pT])
        kb.copy("act", aT[k2][:], pT[:, 0:4, :], [b_pT], [b_aT[k2]])
        for nn in range(2):
            for cc in range(4):
                kb.mm(py[nn][:], aT[k2][:, cc, :], Wd[kw][:, cc, nn * 512:(nn + 1) * 512], cc == 0, cc == 3, [b_aT[k2], b_Wd[kw]], [b_py[nn]])
        kb.copy("act", yr[k2][:, 0:512], py[0][:], [b_py[0]], [b_yr[k2]])
        kb.copy("dve", yr[k2][:, 512:1024], py[1][:], [b_py[1]], [b_yr[k2]])
        kb.P.dma("pool", lambda en, j=j, k2=k2: en.indirect_dma_start(
            out=T["Yd"], out_offset=bass.IndirectOffsetOnAxis(ap=idx[j][:, 0:1], axis=0), in_=yr[k2][:], in_offset=None,
            bounds_check=breg(en, 2 * S - 1), oob_is_err=False), [b_idx[j], b_yr[k2]], [T["b_Yd"]])
        kb.P.end_guard()
        if s_ == NSLOT - 1 and e + 1 < NE:
            cast_expert(e + 1)
    kb.pop()
    kb.push()
    G = kb.sb([128, D], F32, "G2")
    bG = Buf()
    base = l * 6144 + 5120
    kb.dma(G[:], T["modd"][0:1, base:base + 1024].partition_broadcast(128), r=[T["b_modd"]], w=[bG])
    y0 = [kb.sb([128, D], F32, "y0") for _ in range(2)]
    y1 = [kb.sb([128, D], F32, "y1") for _ in range(2)]
    xt = [kb.sb([128, D], F32, "xt3") for _ in range(2)]
    b_in = [Buf(), Buf()]
    acc = [kb.sb([128, D], F32, "acc") for _ in range(2)]
    b_acc = [Buf(), Buf()]
    for i in range(NT):
        k = i % 2
        kb.dma(y0[k][:], T["Yd"][i * 128:(i + 1) * 128, :], r=[T["b_Yd"]], w=[b_in[k]])
        kb.dma(y1[k][:], T["Yd"][S + i * 128:S + (i + 1) * 128, :], r=[T["b_Yd"]], w=[b_in[k]])
        kb.dma(xt[k][:], xsrc[i * 128:(i + 1) * 128, :], r=[bx[i]], w=[b_in[k]])
        kb.ts("dve", acc[k][:], y0[k][:], wts[:, i, 0:1], None, ALU.mult, None, [b_in[k], b_wts], [b_acc[k]])
        kb.stt("dve", acc[k][:], y1[k][:], wts[:, i, 1:2], acc[k][:], ALU.mult, ALU.add, [b_in[k], b_wts, b_acc[k]], [b_acc[k]])
        kb.tt("dve", acc[k][:], acc[k][:], G[:], ALU.mult, [b_acc[k], bG], [b_acc[k]])
        kb.tt("dve", acc[k][:], acc[k][:], xt[k][:], ALU.add, [b_acc[k], b_in[k]], [b_acc[k]])
        kb.dma(xdst[i * 128:(i + 1) * 128, :], acc[k][:], r=[b_acc[k]], w=[bxd[i]])
    kb.pop()
    kb.pop()


def _headnorm_consts(kb, gsrc, extra_scale):
    blk = kb.sb([128, 128], BF16, "blk64")
    b = Buf()
    kb.memset("dve", blk[:], 0.0, [b])
    kb.memset("dve", blk[0:64, 0:64], 1.0, [b])
    kb.memset("dve", blk[64:128, 64:128], 1.0, [b])
    gcol = kb.sb([128, 1], F32, "gcol")
    kb.dma(gcol[0:64, :], gsrc.rearrange("o d -> d o"), w=[b])
    kb.dma(gcol[64:128, :], gsrc.rearrange("o d -> d o"), w=[b])
    if extra_scale != 1.0:
        kb.ts("dve", gcol[:], gcol[:], float(extra_scale), None, ALU.mult, None, [b], [b])
    return blk, gcol, b


class HeadNormT:
    def __init__(self, kb, blk, gcol, bconst):
        self.kb = kb
        self.blk, self.gcol, self.bconst = blk, gcol, bconst
        self.sq = [kb.sb([128, 512], BF16, "hsq") for _ in range(2)]
        self.rn = [kb.sb([128, 512], F32, "hrn") for _ in range(2)]
        self.b = [Buf(), Buf()]
        self.pn = kb.ps([128, 512], F32, "hpn")
        self.b_pn = Buf()
        self.n = 0

    def run(self, out, pin, b_pin, b_out):
        k = self.s1(pin, b_pin)
        self.s2(k)
        self.s3(k, out, pin, b_pin, b_out)

    def s1(self, pin, b_pin):
        k = self.n % 2
        self.n += 1
        self.kb.act(self.sq[k][:], pin, AF.Square, [b_pin], [self.b[k]])
        return k

    def s2(self, k):
        self.kb.mm(self.pn[:], self.blk[:], self.sq[k][:], True, True, [self.b[k], self.bconst], [self.b_pn])

    def s3(self, k, out, pin, b_pin, b_out):
        kb = self.kb
        c = kb.c
        kb.act(self.rn[k][:], self.pn[:], AF.Sqrt, [self.b_pn, kb.bc], [self.b[k]], scale=1.0 / 64, bias=c["epsc"][:])
        kb.op("dve", lambda e: e.reciprocal(out=self.rn[k][:], in_=self.rn[k][:]), [self.b[k]], [self.b[k]])
        kb.stt("dve", out, pin, self.gcol[:, 0:1], self.rn[k][:], ALU.mult, ALU.mult, [b_pin, self.b[k], self.bconst], [b_out])


def phase_kv(kb, T, xsrc, bx):
    c = kb.c
    kb.push()
    A, Bt, G_unused, bmod = load_mod(kb, T, 0, "kv")
    Wkv = kb.sb([128, 8, 2064], BF16, "Wkv")
    bW = Buf()
    load_w_bf16(kb, Wkv, T["kv_w"], D, 2064, bW)
    blk, gcol, bconst = _headnorm_consts(kb, T["k_norm_g"], 1.0)
    hn = HeadNormT(kb, blk, gcol, bconst)
    fb = kb.sb([128, 16], F32, "fb")
    kb.dma(fb[:], T["kv_forget_b"].partition_broadcast(128), w=[bconst])
    nt = NormT(kb, A, Bt, bmod)
    hTg = [kb.sb([128, 8, 512], BF16, "hTg") for _ in range(2)]
    b_hTg = [Buf(), Buf()]
    pk = [kb.ps([128, 512], F32, "pk") for _ in range(3)]
    b_pk = [Buf() for _ in range(3)]
    psm = kb.ps([128, 512], F32, "psm")
    b_psm = Buf()
    kst = [kb.sb([128, 512], BF16, "kst") for _ in range(2)]
    b_kst = [Buf(), Buf()]
    vst = [kb.sb([128, 16, 128], BF16, "vst") for _ in range(2)]
    b_vst = [Buf(), Buf()]
    for j in range(2):
        kb.memset("dve", vst[j][:], 1.0, [b_vst[j]])
    carry = kb.sb([128, 16], F32, "carry")
    b_carry = Buf()
    kb.memset("dve", carry[:], 0.0, [b_carry])
    lf = [kb.sb([128, 16], F32, "lf") for _ in range(2)]
    fcm = [kb.sb([128, 16], F32, "fcm") for _ in range(2)]
    r1 = [kb.sb([128, 16], F32, "r1") for _ in range(2)]
    sp3 = [kb.sb([128, 2, 3, 16], BF16, "sp3") for _ in range(2)]
    spT = [kb.sb([48, 2, 128], BF16, "spT") for _ in range(2)]
    b_f = [Buf(), Buf()]
    pst = kb.ps([48, 2, 128], BF16, "pst")
    b_pst = Buf()
    onesr = kb.sb([48, 512], BF16, "onesr")
    kb.memset("dve", onesr[:], 1.0, [bconst])
    for r in range(3):
        for g in range(S // 512):
            kb.dma(T["kaug"][:, 64 + r, g * 512:(g + 1) * 512], onesr[0:16, :], r=[bconst], w=[T["b_kaug"]])
            kb.dma(T["qaug"][:, 67 + r, g * 512:(g + 1) * 512], onesr[0:16, :], r=[bconst], w=[T["b_qaug"]])
    nv = 0
    NG = S // 512
    kcnt = [0]

    def kchunk_gen(g, ch):
        kg = g % 2
        idx = kcnt[0]
        kcnt[0] += 1
        k3 = idx % 3
        k2 = idx % 2

        def gen():
            for cc in range(8):
                kb.mm(pk[k3][:], Wkv[:, cc, ch * 128:(ch + 1) * 128], hTg[kg][:, cc, :], cc == 0, cc == 7, [bW, b_hTg[kg]], [b_pk[k3]])
            yield
            kk = hn.s1(pk[k3][:], b_pk[k3])
            yield
            hn.s2(kk)
            yield
            hn.s3(kk, kst[k2][:], pk[k3][:], b_pk[k3], b_kst[k2])
            kb.dma(T["kaug"][2 * ch, 0:64, g * 512:(g + 1) * 512], kst[k2][0:64, :], r=[b_kst[k2]], w=[T["b_kaug"]])
            kb.dma(T["kaug"][2 * ch + 1, 0:64, g * 512:(g + 1) * 512], kst[k2][64:128, :], r=[b_kst[k2]], w=[T["b_kaug"]])
        return gen

    for t in range(4):
        nt.run(xsrc[t * 128:(t + 1) * 128, :], bx[t], hTg[0][:, :, t * 128:(t + 1) * 128], b_hTg[0])
    for g in range(NG):
        kg = g % 2
        pend = {}

        def extra(r, g=g):
            if g + 1 >= NG:
                return
            t, ph = divmod(r, 2)
            if t < 4 and ph == 0:
                i = (g + 1) * 4 + t
                pend[t] = nt.run_a(xsrc[i * 128:(i + 1) * 128, :], bx[i])
            if t < 4 and ph == 1:
                kn = (g + 1) % 2
                nt.run_b(pend[t], hTg[kn][:, :, t * 128:(t + 1) * 128], b_hTg[kn])

        pipeline([kchunk_gen(g, ch) for ch in range(8)], extra)
        for t in range(4):
            i = g * 4 + t
            kv = nv % 2
            nv += 1
            for n in range(2):
                for cc in range(8):
                    kb.mm(pk[n][:], hTg[kg][:, cc, t * 128:(t + 1) * 128], Wkv[:, cc, 1024 + n * 512:1024 + (n + 1) * 512],
                          cc == 0, cc == 7, [b_hTg[kg], bW], [b_pk[n]])
            for cc in range(8):
                kb.mm(psm[:, 0:16], hTg[kg][:, cc, t * 128:(t + 1) * 128], Wkv[:, cc, 2048:2064], cc == 0, cc == 7, [b_hTg[kg], bW], [b_psm])
            for n in range(2):
                eng = "act" if n == 0 else "dve"
                kb.copy(eng, vst[kv][:, n * 8:(n + 1) * 8, 0:64], pk[n][:].rearrange("p (h d) -> p h d", h=8), [b_pk[n]], [b_vst[kv]])
            kb.dma(T["vaug"][:, i * 128:(i + 1) * 128, :].rearrange("h t d -> t h d"), vst[kv][:], r=[b_vst[kv]], w=[T["b_vaug"]])
            B = [b_f[kv]]
            kb.tt("dve", lf[kv][:], psm[:, 0:16], fb[:], ALU.add, [b_psm, bconst], B)
            kb.act(lf[kv][:], lf[kv][:], AF.Exp, B, B, scale=-1.0)
            kb.act(lf[kv][:], lf[kv][:], AF.Ln, B + [kb.bc], B, bias=c["onec"][:])
            kb.ts("dve", lf[kv][:], lf[kv][:], -1.0, None, ALU.mult, None, B, B)
            kb.mm(psm[:, 16:32], c["utri"][:], lf[kv][:], True, True, B + [kb.bc], [b_psm])
            kb.mm(psm[:, 32:48], c["onesf"][:], lf[kv][:], True, True, B + [kb.bc], [b_psm])
            kb.tt("dve", fcm[kv][:], psm[:, 16:32], carry[:], ALU.add, [b_psm, b_carry], B)
            kb.tt("dve", carry[:], carry[:], psm[:, 32:48], ALU.add, [b_psm, b_carry], [b_carry])
            kb.dma(T["fcum_d"][i * 128:(i + 1) * 128, :], fcm[kv][:], r=B, w=[T["b_fcum"]])
            kb.copy("dve", sp3[kv][:, 0, 0, :], fcm[kv][:], B, B)
            kb.tt("dve", r1[kv][:], fcm[kv][:], sp3[kv][:, 0, 0, :], ALU.subtract, B, B)
            kb.copy("dve", sp3[kv][:, 0, 1, :], r1[kv][:], B, B)
            kb.tt("dve", r1[kv][:], r1[kv][:], sp3[kv][:, 0, 1, :], ALU.subtract, B, B)
            kb.copy("dve", sp3[kv][:, 0, 2, :], r1[kv][:], B, B)
            kb.ts("dve", sp3[kv][:, 1, :, :], sp3[kv][:, 0, :, :], -1.0, None, ALU.mult, None, B, B)
            for qk in range(2):
                kb.tr(pst[:, qk, :], sp3[kv][:, qk, :, :].rearrange("p r h -> p (r h)"), c["idb"][:], B + [kb.bc], [b_pst])
            kb.copy("act", spT[kv][:], pst[:], [b_pst], B)
            for r in range(3):
                kb.dma(T["qaug"][:, 64 + r, i * 128:(i + 1) * 128], spT[kv][r * 16:(r + 1) * 16, 0, :], r=B, w=[T["b_qaug"]])
                kb.dma(T["kaug"][:, 67 + r, i * 128:(i + 1) * 128], spT[kv][r * 16:(r + 1) * 16, 1, :], r=B, w=[T["b_kaug"]])
    kb.pop()


def phase_fox_proj(kb, T, l, xsrc, bx):
    c = kb.c
    j = l - 2
    kb.push()
    A, Bt, G_unused, bmod = load_mod(kb, T, l, "mix")
    Wqz = kb.sb([128, 8, 2048], BF16, "Wqz")
    bW = Buf()
    load_w_bf16(kb, Wqz, T["fox_w_qz"][j], D, 2048, bW)
    blk, gcol, bconst = _headnorm_consts(kb, T["fox_q_norm_g"][j:j + 1, :], 64.0 ** -0.5)
    hn = HeadNormT(kb, blk, gcol, bconst)
    nt = NormT(kb, A, Bt, bmod)
    hTg = [kb.sb([128, 8, 512], BF16, "hTg") for _ in range(2)]
    b_hTg = [Buf(), Buf()]
    pq = [kb.ps([128, 512], F32, "pq") for _ in range(3)]
    b_pq = [Buf() for _ in range(3)]
    qst = [kb.sb([128, 512], BF16, "qst") for _ in range(4)]
    b_qst = [Buf() for _ in range(4)]
    NG = S // 512
    cnt = [0]

    def chunk_gen(g, ch):
        kg = g % 2
        idx = cnt[0]
        cnt[0] += 1
        k3 = idx % 3
        k4 = idx % 4

        def gen():
            for cc in range(8):
                kb.mm(pq[k3][:], Wqz[:, cc, ch * 128:(ch + 1) * 128], hTg[kg][:, cc, :], cc == 0, cc == 7, [bW, b_hTg[kg]], [b_pq[k3]])
            yield
            if ch >= 8:
                kb.act(qst[k4][:], pq[k3][:], AF.Sigmoid, [b_pq[k3]], [b_qst[k4]])
                kb.dma(T["szT"][ch - 8, :, g * 512:(g + 1) * 512], qst[k4][:], r=[b_qst[k4]], w=[T["b_szT"]])
                return
            kk = hn.s1(pq[k3][:], b_pq[k3])
            yield
            hn.s2(kk)
            yield
            hn.s3(kk, qst[k4][:], pq[k3][:], b_pq[k3], b_qst[k4])
            kb.dma(T["qaug"][2 * ch, 0:64, g * 512:(g + 1) * 512], qst[k4][0:64, :], r=[b_qst[k4]], w=[T["b_qaug"]])
            kb.dma(T["qaug"][2 * ch + 1, 0:64, g * 512:(g + 1) * 512], qst[k4][64:128, :], r=[b_qst[k4]], w=[T["b_qaug"]])
        return gen

    for t in range(4):
        nt.run(xsrc[t * 128:(t + 1) * 128, :], bx[t], hTg[0][:, :, t * 128:(t + 1) * 128], b_hTg[0])
    for g in range(NG):
        pend = {}

        def extra(r, g=g):
            if g + 1 >= NG:
                return
            t, ph = divmod(r, 4)
            if t < 4 and ph == 1:
                i = (g + 1) * 4 + t
                pend[t] = nt.run_a(xsrc[i * 128:(i + 1) * 128, :], bx[i])
            if t < 4 and ph == 3:
                kn = (g + 1) % 2
                nt.run_b(pend[t], hTg[kn][:, :, t * 128:(t + 1) * 128], b_hTg[kn])

        pipeline([chunk_gen(g, ch) for ch in range(16)], extra)
    kb.pop()


def phase_fox_attn(kb, T, l):
    c = kb.c
    kb.push()
    ka = [kb.sb([70, S], BF16, "ka") for _ in range(2)]
    qa = [kb.sb([70, S], BF16, "qa") for _ in range(2)]
    va = [kb.sb([128, NT, 128], BF16, "va") for _ in range(2)]
    sz = [kb.sb([64, S], BF16, "sz") for _ in range(2)]
    b_in = [Buf(), Buf()]
    ps_ = [kb.ps([128, 512], F32, "ps") for _ in range(4)]
    b_ps = [Buf() for _ in range(4)]
    po = [kb.ps([128, 512], F32, "po") for _ in range(2)]
    b_po = [Buf(), Buf()]
    pT = [kb.sb([128, 512], BF16, "pT") for _ in range(4)]
    b_pT = [Buf() for _ in range(4)]
    rl = [kb.sb([128, 512], F32, "rl") for _ in range(2)]
    on = [kb.sb([64, 512], F32, "on") for _ in range(2)]
    og = [kb.sb([64, 512], BF16, "og") for _ in range(2)]
    b_o = [Buf(), Buf()]

    def loads(h):
        k = h % 2
        kb.dma(ka[k][:], T["kaug"][h], r=[T["b_kaug"]], w=[b_in[k]])
        kb.dma(qa[k][:], T["qaug"][h], r=[T["b_qaug"]], w=[b_in[k]])
        kb.dma(va[k][:], T["vaug"][h].rearrange("(t p) d -> p t d", p=128), r=[T["b_vaug"]], w=[b_in[k]])
        ch, half = divmod(h, 2)
        kb.dma(sz[k][:], T["szT"][ch, half * 64:(half + 1) * 64, :], r=[T["b_szT"]], w=[b_in[k]])

    loads(0)
    LA = 3
    ng = 0
    n = 0
    for h in range(FH):
        k = h % 2
        if h + 1 < FH:
            loads(h + 1)
        pairs = []
        for g in range(S // 512):
            nk = 4 * g + 4
            for kt in range(nk):
                pairs.append((g, kt, nk))
        kgs = {}
        for g in range(S // 512):
            kgs[g] = ng % 2
            ng += 1
        slot = {}

        def emit_s(pi):
            g, kt, nk = pairs[pi]
            k3 = (n + pi) % 4
            d = kt - 4 * g
            q0 = g * 512 + (d * 128 if d > 0 else 0)
            w = (g + 1) * 512 - q0
            diag = d >= 0
            kb.mm(ps_[k3][:, 0:w], ka[k][:, kt * 128:(kt + 1) * 128], qa[k][:, q0:q0 + w], True, not diag, [b_in[k]], [b_ps[k3]])
            if diag:
                kb.mm(ps_[k3][:, 0:128], c["idb"][:], c["negmb"][:], False, True, [kb.bc], [b_ps[k3]])
            kb.act(pT[k3][:, 0:w], ps_[k3][:, 0:w], AF.Exp, [b_ps[k3]], [b_pT[k3]])

        def emit_o(pi):
            g, kt, nk = pairs[pi]
            k3 = (n + pi) % 4
            kg = kgs[g]
            d = kt - 4 * g
            q0 = g * 512 + (d * 128 if d > 0 else 0)
            w = (g + 1) * 512 - q0
            c0 = q0 - g * 512
            kb.mm(po[kg][:, c0:c0 + w], va[k][:, kt, :], pT[k3][:, 0:w], kt == 0, kt == nk - 1, [b_in[k], b_pT[k3]], [b_po[kg]])
            if kt == nk - 1:
                kb.op("dve", lambda e, kg=kg: e.reciprocal(out=rl[kg][64:128, :], in_=po[kg][64:128, :]), [b_po[kg]], [b_o[kg]])
                kb.tt("dve", on[kg][:], po[kg][0:64, :], rl[kg][64:128, :], ALU.mult, [b_po[kg], b_o[kg]], [b_o[kg]])
                kb.tt("dve", og[kg][:], on[kg][:], sz[k][:, g * 512:(g + 1) * 512], ALU.mult, [b_o[kg], b_in[k]], [b_o[kg]])
                kb.dma(T["ogT"][h * 64:(h + 1) * 64, g * 512:(g + 1) * 512], og[kg][:], r=[b_o[kg]], w=[T["b_ogT"]])

        npairs = len(pairs)
        for pi in range(npairs + LA):
            if pi < npairs:
                emit_s(pi)
            if pi - LA >= 0:
                emit_o(pi - LA)
        n += npairs
    kb.pop()


def phase_fox_out(kb, T, l, xsrc, bx, xdst, bxd):
    c = kb.c
    j = l - 2
    kb.push()
    G = kb.sb([128, D], F32, "G")
    bG = Buf()
    base = l * 6144 + 2048
    kb.dma(G[:], T["modd"][0:1, base:base + 1024].partition_broadcast(128), r=[T["b_modd"]], w=[bG])
    Wo = kb.sb([128, 8, D], BF16, "Wo")
    bW = Buf()
    load_w_bf16(kb, Wo, T["fox_w_out"][j], D, D, bW)
    ot = [kb.sb([128, 8, 128], BF16, "ot") for _ in range(2)]
    xt = [kb.sb([128, D], F32, "xt") for _ in range(2)]
    b_in = [Buf(), Buf()]
    py = [kb.ps([128, 512], F32, "py") for _ in range(2)]
    b_py = [Buf(), Buf()]
    yt = [kb.sb([128, D], F32, "yt") for _ in range(2)]
    b_yt = [Buf(), Buf()]
    for i in range(NT):
        k = i % 2
        kb.dma(ot[k][:], T["ogT"][:, i * 128:(i + 1) * 128].rearrange("(c p) s -> p c s", p=128), r=[T["b_ogT"]], w=[b_in[k]])
        kb.dma(xt[k][:], xsrc[i * 128:(i + 1) * 128, :], r=[bx[i]], w=[b_in[k]])
        for n in range(2):
            for cc in range(8):
                kb.mm(py[n][:], ot[k][:, cc, :], Wo[:, cc, n * 512:(n + 1) * 512], cc == 0, cc == 7, [b_in[k], bW], [b_py[n]])
            kb.tt("dve", yt[k][:, n * 512:(n + 1) * 512], py[n][:], G[:, n * 512:(n + 1) * 512], ALU.mult, [b_py[n], bG], [b_yt[k]])
        kb.tt("dve", yt[k][:], yt[k][:], xt[k][:], ALU.add, [b_yt[k], b_in[k]], [b_yt[k]])
        kb.dma(xdst[i * 128:(i + 1) * 128, :], yt[k][:], r=[b_yt[k]], w=[bxd[i]])
    kb.pop()


IN_SHAPES = {
    "x": [S, D], "c": [1, D], "mod_w": [4, D, 6144], "mod_b": [4, 6144], "norm_mix_g": [4, D], "norm_ffn_g": [4, D],
    "gdn_w_in": [2, D, GPROJ], "gdn_conv_w": [2, 4, 3072], "gdn_a_log": [2, 8], "gdn_dt_bias": [2, 8],
    "gdn_out_norm_g": [2, 128], "gdn_w_out": [2, D, D], "kv_mod_w": [D, 2048], "kv_mod_b": [1, 2048],
    "kv_norm_g": [1, D], "kv_w": [D, 2064], "kv_forget_b": [1, 16], "k_norm_g": [1, 64],
    "fox_w_qz": [2, D, 2048], "fox_q_norm_g": [2, 64], "fox_w_out": [2, D, D],
    "moe_w_group": [4, D, 4], "moe_b_group": [4, 4], "moe_w_expert": [4, D, 32], "moe_b_expert": [4, 32],
    "moe_w_gate": [4, 32, D, DE], "moe_w_up": [4, 32, D, DE], "moe_w_down": [4, 32, DE, D],
}


class LazyT(dict):
    def __init__(self, kb, lazy):
        super().__init__()
        self.kb = kb
        self.lazy = lazy
        self.declared = []

    def __missing__(self, k):
        if k in IN_SHAPES:
            v = self.kb.din(k, IN_SHAPES[k])
            self[k] = v
            self.declared.append(k)
            return v
        raise KeyError(k)


def build(stop=None, dbg=(), lazy=False, stop2=None):
    kb = KB(dbg)
    T = LazyT(kb, lazy)
    kb.T = T
    if not lazy:
        for k in IN_SHAPES:
            T[k]
    T["y"] = kb.dout("y", [S, D])
    T["b_y"] = [Buf() for _ in range(NT)]
    T["modd"] = kb.dscr("modd", [1, 6144 * 4 + 2048])
    T["b_modd"] = Buf()
    T["xs"] = kb.dscr("xs", [S, D])
    T["b_xs"] = [Buf() for _ in range(NT)]
    T["b_xin"] = [Buf() for _ in range(NT)]
    T["qkv_d"] = kb.dscr("qkv_d", [NT, 128, 24 * 128], BF16)
    T["b_qkv"] = [Buf() for _ in range(NT)]
    T["sz_d"] = kb.dscr("sz_d", [S, D], BF16)
    T["b_sz"] = [Buf() for _ in range(NT)]
    T["rowasg"] = kb.dscr("rowasg", [NE * CAP, 2], I32)
    T["b_rowasg"] = Buf()
    T["cnt_d"] = kb.dscr("cnt_d", [1, 32], I32)
    T["b_cntd"] = Buf()
    T["hd"] = kb.dscr("hd", [2 * S, D], BF16)
    T["b_hd"] = Buf()
    T["Yd"] = kb.dscr("Yd", [2 * S, D], F32)
    T["b_Yd"] = Buf()
    T["kaug"] = kb.dscr("kaug", [FH, 70, S], BF16)
    T["b_kaug"] = Buf()
    T["qaug"] = kb.dscr("qaug", [FH, 70, S], BF16)
    T["b_qaug"] = Buf()
    T["vaug"] = kb.dscr("vaug", [FH, S, 128], BF16)
    T["b_vaug"] = Buf()
    T["fcum_d"] = kb.dscr("fcum_d", [S, 16], F32)
    T["b_fcum"] = Buf()
    T["szT"] = kb.dscr("szT", [8, 128, S], BF16)
    T["b_szT"] = Buf()
    T["ogT"] = kb.dscr("ogT", [D, S], BF16)
    T["b_ogT"] = Buf()
    T["gb_d"] = kb.dscr("gb_d", [S, 16], F32)
    T["b_gb"] = [Buf() for _ in range(NT)]
    kb.make_consts()
    if stop == "kvtest":
        phase_mod(kb, T)
        phase_kv(kb, T, T["x"], T["b_xin"])
        if dbg and "kaug" in dbg and stop2 == "kv":
            return finish(kb, T)
        phase_fox_proj(kb, T, 2, T["x"], T["b_xin"])
        phase_fox_attn(kb, T, 2)
        phase_fox_out(kb, T, 2, T["x"], T["b_xin"], T["xs"], T["b_xs"])
        return finish(kb, T, dump_x=True)
    phase_mod(kb, T)
    if stop == "mod":
        return finish(kb, T)
    xsrc, bx = T["x"], T["b_xin"]
    for l in range(DEPTH):
        last = l == DEPTH - 1
        if l < 2:
            phase_gdn_proj(kb, T, l, xsrc, bx)
            if stop == "gdnproj":
                return finish(kb, T)
            phase_gdn_delta(kb, T, l, xsrc, bx, T["xs"], T["b_xs"])
        else:
            phase_fox_proj(kb, T, l, xsrc, bx)
            phase_fox_attn(kb, T, l)
            phase_fox_out(kb, T, l, xsrc, bx, T["xs"], T["b_xs"])
        xsrc, bx = T["xs"], T["b_xs"]
        if stop == "mix%d" % l:
            return finish(kb, T, dump_x=True)
        if last:
            phase_moe(kb, T, l, xsrc, bx, T["y"], T["b_y"])
        else:
            phase_moe(kb, T, l, xsrc, bx, T["xs"], T["b_xs"])
        if stop == "moe%d" % l:
            return finish(kb, T, dump_x=True)
        if l == 1:
            phase_kv(kb, T, xsrc, bx)
    return finish(kb, T)


def finish(kb, T, dump_x=False):
    global LASTP, LASTT
    P = kb.P
    LASTP = P
    LASTT = T
    if dump_x:
        for i in range(NT):
            kb.dma(T["y"][i * 128:(i + 1) * 128, :], T["xs"][i * 128:(i + 1) * 128, :], r=[T["b_xs"][i]], w=[Buf()])
    P.barrier()
    P.check()
    P.emit()
    P.close()
    while kb.scopes:
        for cm in reversed(kb.scopes.pop()):
            cm.__exit__(None, None, None)
    return kb.nc


_NC_CACHE = {}


def kernel(**inputs):
    if "nc" not in _NC_CACHE:
        _REGS.clear()
        _NC_CACHE["nc"] = build()
    nc = _NC_CACHE["nc"]
    ncores = 8
    shared = {}
    for k, shp in IN_SHAPES.items():
        if k in ("x", "c"):
            continue
        shared[k] = np.ascontiguousarray(np.asarray(inputs[k], dtype=np.float32).reshape(shp))
    x = np.asarray(inputs["x"], dtype=np.float32)
    cc = np.asarray(inputs["c"], dtype=np.float32)
    in_maps = []
    for b in range(ncores):
        m = dict(shared)
        m["x"] = np.ascontiguousarray(x[b])
        m["c"] = np.ascontiguousarray(cc[b:b + 1])
        in_maps.append(m)
    res = run_bass_kernel_spmd(nc, in_maps, core_ids=list(range(ncores)))
    out = np.stack([np.asarray(r["y"], dtype=np.float32) for r in res.results], axis=0)
    return out
```
